# Optimizing a Trainium2 kernel written in Bass

```python
import math
import jax, jax.numpy as jnp
from jax import lax
import numpy as np

D_MODEL = 2048
BATCH = 2
SEQ = 8192
DEPTH = 1

EPS = 1e-6
Q_BLOCK = 128

DA_HEADS = 8
DA_QK_DIM = 64
DA_V_DIM = 128

DL_GROUPS = ((128, 1), (512, 4), (2048, 16))
DL_N_GROUPS = 3
DL_HEADS_PER_GROUP = 4
DL_HEAD_DIM = 128

DA_Q_COLS = DA_HEADS * 2 * DA_QK_DIM
DA_K_COLS = DA_HEADS * 2 * DA_QK_DIM
DA_V_COLS = DA_HEADS * DA_V_DIM
DL_COLS = DL_N_GROUPS * DL_HEADS_PER_GROUP * DL_HEAD_DIM
GATE_COLS = D_MODEL
N_IN = DA_Q_COLS + DA_K_COLS + DA_V_COLS + 3 * DL_COLS + 2 * GATE_COLS
DA_OUT = DA_HEADS * DA_V_DIM
DL_OUT = DL_HEADS_PER_GROUP * DL_HEAD_DIM

MOE_GROUPS = 4
MOE_EXPERTS_PER_GROUP = 8
MOE_N_EXPERTS = MOE_GROUPS * MOE_EXPERTS_PER_GROUP
MOE_TOP_K = 2
MOE_D_FF = 1024
MOE_BLOCK = 128

kernel_name = "hybrid_diffattn_dilated_hiermoe_block"


def rmsnorm(t, gain):
    tf = t.astype(jnp.float32)
    tf = tf * lax.rsqrt(jnp.mean(tf * tf, axis=-1, keepdims=True) + EPS)
    return (tf * gain.astype(jnp.float32)).astype(t.dtype)


def alibi_slopes(n):
    return jnp.asarray(np.array([2.0 ** (-8.0 * (h + 1) / n) for h in range(n)], dtype=np.float32))


def diff_attention(q, k, v, lam, lam_init, sub_gain, slopes):
    B, S, H, _, dk = q.shape
    dv = v.shape[-1]
    nblk = S // Q_BLOCK
    scale = dk ** -0.5
    kpos = jnp.arange(S)
    qb = q.reshape(B, nblk, Q_BLOCK, H, 2, dk).transpose(1, 0, 2, 3, 4, 5)

    def block(args):
        i, qblk = args
        qpos = i * Q_BLOCK + jnp.arange(Q_BLOCK)
        dist = (qpos[:, None] - kpos[None, :]).astype(jnp.float32)
        s = jnp.einsum('bqhmd,bkhmd->bhmqk', qblk, k).astype(jnp.float32) * scale
        s = s - slopes[None, :, None, None, None] * dist
        s = jnp.where(dist >= 0, s, -jnp.inf)
        p = jax.nn.softmax(s, axis=-1)
        a = p[:, :, 0] - lam * p[:, :, 1]
        return jnp.einsum('bhqk,bkhd->bqhd', a.astype(v.dtype), v)

    o = lax.map(block, (jnp.arange(nblk), qb))
    o = o.transpose(1, 0, 2, 3, 4).reshape(B, S, H, dv)
    return rmsnorm(o, sub_gain) * (1.0 - lam_init)


def dilated_group(q, k, v, window, dilation, slopes):
    B, S, H, dh = q.shape
    span = window // dilation
    L = S // dilation
    nb = -(-L // span)
    Lp = nb * span

    def to_sub(t):
        t = t.reshape(B, L, dilation, H, dh).transpose(0, 2, 1, 3, 4)
        t = jnp.pad(t, ((0, 0), (0, 0), (0, Lp - L), (0, 0), (0, 0)))
        return t.reshape(B, dilation, nb, span, H, dh)

    def with_prev(t):
        prev = jnp.pad(t, ((0, 0), (0, 0), (1, 0), (0, 0), (0, 0), (0, 0)))[:, :, :-1]
        return jnp.concatenate([prev, t], axis=3)

    qs = to_sub(q)
    kc = with_prev(to_sub(k))
    vc = with_prev(to_sub(v))
    s = jnp.einsum('brnqhd,brnkhd->brnhqk', qs, kc).astype(jnp.float32) * (dh ** -0.5)
    qi = jnp.arange(span)
    kj = jnp.arange(2 * span)
    step = qi[:, None] + span - kj[None, :]
    blk = jnp.arange(nb)
    valid = (step >= 0) & (step <= span) & ((blk[:, None, None] * span - span + kj[None, None, :]) >= 0)
    s = s - slopes[:, None, None] * (dilation * step).astype(jnp.float32)
    s = jnp.where(valid[:, None], s, -jnp.inf)
    m = jnp.max(s, axis=-1, keepdims=True)
    p = jnp.exp(s - m)
    den = jnp.sum(p, axis=-1, keepdims=True)
    o = jnp.einsum('brnhqk,brnkhd->brnqhd', (p / den).astype(v.dtype), vc)
    lse = (m + jnp.log(den))[..., 0]
    o = o.reshape(B, dilation, Lp, H, dh)[:, :, :L].transpose(0, 2, 1, 3, 4).reshape(B, S, H, dh)
    lse = lse.transpose(0, 1, 2, 4, 3).reshape(B, dilation, Lp, H)[:, :, :L]
    lse = lse.transpose(0, 2, 1, 3).reshape(B, S, H)
    return o, lse


def hier_moe(h, w_group_router, w_expert_router, w_gate_up, w_down):
    B, S, D = h.shape
    T = B * S
    TK = T * MOE_TOP_K
    xt = h.reshape(T, D)
    g_prob = jax.nn.softmax((xt @ w_group_router).astype(jnp.float32), axis=-1)
    g_w, g_idx = lax.top_k(g_prob, 1)
    e_logits = (xt @ w_expert_router).astype(jnp.float32).reshape(T, MOE_GROUPS, MOE_EXPERTS_PER_GROUP)
    e_in_group = jnp.take_along_axis(e_logits, g_idx[:, :, None], axis=1)[:, 0]
    e_top, e_local = lax.top_k(e_in_group, MOE_TOP_K)
    e_w = jax.nn.softmax(e_top, axis=-1) * g_w
    e_idx = g_idx * MOE_EXPERTS_PER_GROUP + e_local

    flat_e = e_idx.reshape(-1)
    order = jnp.argsort(flat_e)
    sorted_e = flat_e[order]
    tok = order // MOE_TOP_K
    sizes = jnp.bincount(flat_e, length=MOE_N_EXPERTS).astype(jnp.int32)
    start = jnp.cumsum(sizes) - sizes
    padded = ((sizes + MOE_BLOCK - 1) // MOE_BLOCK) * MOE_BLOCK
    pad_end = jnp.cumsum(padded)
    pad_start = pad_end - padded
    dest = pad_start[sorted_e] + (jnp.arange(TK) - start[sorted_e])
    n_blocks = -(-TK // MOE_BLOCK) + MOE_N_EXPERTS
    x_pad = jnp.zeros((n_blocks * MOE_BLOCK, D), xt.dtype).at[dest].set(xt[tok])
    blk_expert = jnp.minimum(
        jnp.searchsorted(pad_end, jnp.arange(n_blocks) * MOE_BLOCK, side='right'),
        MOE_N_EXPERTS - 1)

    def expert_block(args):
        e, xb = args
        gate, up = jnp.split(xb @ w_gate_up[e], 2, axis=-1)
        return (jax.nn.silu(gate) * up) @ w_down[e]

    y_pad = lax.map(expert_block, (blk_expert, x_pad.reshape(n_blocks, MOE_BLOCK, D)))
    ys = y_pad.reshape(n_blocks * MOE_BLOCK, D)[dest]
    ys = ys * e_w.reshape(-1)[order][:, None].astype(ys.dtype)
    y = jnp.zeros_like(xt).at[tok].add(ys)
    return y.reshape(B, S, D)


def setup_inputs(seed: int = 0) -> dict:
    key = jax.random.key(seed)
    ks = jax.random.split(key, 20)
    f32 = jnp.float32

    def nrm(k, shape, scale):
        return jax.random.normal(k, shape, f32) * scale

    def gain(k, shape):
        return 1.0 + 0.05 * jax.random.normal(k, shape, f32)

    return {
        "x": jax.random.normal(ks[0], (BATCH, SEQ, D_MODEL), f32),
        "norm_mix": gain(ks[1], (DEPTH, D_MODEL)),
        "w_in": nrm(ks[2], (DEPTH, D_MODEL, N_IN), D_MODEL ** -0.5),
        "da_q_norm": gain(ks[3], (DEPTH, DA_QK_DIM)),
        "da_k_norm": gain(ks[4], (DEPTH, DA_QK_DIM)),
        "da_lambda_q": nrm(ks[5], (DEPTH, 2, DA_QK_DIM), 0.1),
        "da_lambda_k": nrm(ks[6], (DEPTH, 2, DA_QK_DIM), 0.1),
        "da_sub_norm": gain(ks[7], (DEPTH, DA_V_DIM)),
        "dl_q_norm": gain(ks[8], (DEPTH, DL_HEAD_DIM)),
        "dl_k_norm": gain(ks[9], (DEPTH, DL_HEAD_DIM)),
        "w_branch_a": nrm(ks[10], (DEPTH, DA_OUT, D_MODEL), DA_OUT ** -0.5),
        "w_branch_b": nrm(ks[11], (DEPTH, DL_OUT, D_MODEL), DL_OUT ** -0.5),
        "w_out": nrm(ks[12], (DEPTH, D_MODEL, D_MODEL), D_MODEL ** -0.5),
        "norm_ffn": gain(ks[13], (DEPTH, D_MODEL)),
        "w_group_router": nrm(ks[14], (DEPTH, D_MODEL, MOE_GROUPS), D_MODEL ** -0.5),
        "w_expert_router": nrm(ks[15], (DEPTH, D_MODEL, MOE_N_EXPERTS), D_MODEL ** -0.5),
        "w_gate_up": nrm(ks[16], (DEPTH, MOE_N_EXPERTS, D_MODEL, 2 * MOE_D_FF), D_MODEL ** -0.5),
        "w_down": nrm(ks[17], (DEPTH, MOE_N_EXPERTS, MOE_D_FF, D_MODEL), MOE_D_FF ** -0.5),
    }


def reference(x, norm_mix, w_in, da_q_norm, da_k_norm, da_lambda_q, da_lambda_k, da_sub_norm,
              dl_q_norm, dl_k_norm, w_branch_a, w_branch_b, w_out, norm_ffn,
              w_group_router, w_expert_router, w_gate_up, w_down):
    B, S, D = x.shape
    da_slopes = alibi_slopes(DA_HEADS)
    dl_slopes = alibi_slopes(DL_N_GROUPS * DL_HEADS_PER_GROUP)
    offs = np.cumsum([0, DA_Q_COLS, DA_K_COLS, DA_V_COLS, DL_COLS, DL_COLS, DL_COLS, GATE_COLS, GATE_COLS])

    for l in range(DEPTH):
        lam_init = 0.8 - 0.6 * math.exp(-0.3 * l)
        h = rmsnorm(x, norm_mix[l])
        proj = h @ w_in[l]
        cols = [proj[..., int(offs[i]):int(offs[i + 1])] for i in range(8)]
        da_q = rmsnorm(cols[0].reshape(B, S, DA_HEADS, 2, DA_QK_DIM), da_q_norm[l])
        da_k = rmsnorm(cols[1].reshape(B, S, DA_HEADS, 2, DA_QK_DIM), da_k_norm[l])
        da_v = cols[2].reshape(B, S, DA_HEADS, DA_V_DIM)
        dl_q = rmsnorm(cols[3].reshape(B, S, DL_N_GROUPS, DL_HEADS_PER_GROUP, DL_HEAD_DIM), dl_q_norm[l])
        dl_k = rmsnorm(cols[4].reshape(B, S, DL_N_GROUPS, DL_HEADS_PER_GROUP, DL_HEAD_DIM), dl_k_norm[l])
        dl_v = cols[5].reshape(B, S, DL_N_GROUPS, DL_HEADS_PER_GROUP, DL_HEAD_DIM)
        gate_a, gate_b = cols[6], cols[7]

        lq = da_lambda_q[l].astype(jnp.float32)
        lk = da_lambda_k[l].astype(jnp.float32)
        lam = jnp.exp(jnp.sum(lq[0] * lk[0])) - jnp.exp(jnp.sum(lq[1] * lk[1])) + lam_init
        o_a = diff_attention(da_q, da_k, da_v, lam, lam_init, da_sub_norm[l], da_slopes)
        o_a = o_a.reshape(B, S, DA_OUT)

        outs, lses = [], []
        for g, (window, dilation) in enumerate(DL_GROUPS):
            o_g, lse_g = dilated_group(dl_q[:, :, g], dl_k[:, :, g], dl_v[:, :, g], window, dilation,
                                       dl_slopes[g * DL_HEADS_PER_GROUP:(g + 1) * DL_HEADS_PER_GROUP])
            outs.append(o_g)
            lses.append(lse_g)
        w_grp = jax.nn.softmax(jnp.stack(lses, axis=0), axis=0)
        o_b = jnp.einsum('gbsh,gbshd->bshd', w_grp.astype(x.dtype), jnp.stack(outs, axis=0))
        o_b = o_b.reshape(B, S, DL_OUT)

        mixed = jax.nn.sigmoid(gate_a) * (o_a @ w_branch_a[l]) + jax.nn.sigmoid(gate_b) * (o_b @ w_branch_b[l])
        x = x + mixed @ w_out[l]

        x = x + hier_moe(rmsnorm(x, norm_ffn[l]), w_group_router[l], w_expert_router[l],
                         w_gate_up[l], w_down[l])
    return x
```

```python
import math
from contextlib import ExitStack

import ml_dtypes
import numpy as np

import concourse.bass as bass
import concourse.mybir as mybir
from concourse.bass_utils import run_bass_kernel_spmd

F32 = mybir.dt.float32
F32R = mybir.dt.float32r
BF16 = mybir.dt.bfloat16
I32 = mybir.dt.int32
U32 = mybir.dt.uint32
AF = mybir.ActivationFunctionType
ALU = mybir.AluOpType
AX = mybir.AxisListType

D = 2048
S_LEN = 8192
NT = 64
NOWN = 16
N_IN = 11776
EPS = 1e-6
C_DAQ, C_DAK, C_DAV, C_DLQ, C_DLK, C_DLV, C_GA, C_GB = 0, 1024, 2048, 3072, 4608, 6144, 7680, 9728
NEXP = 32
CAP = 256
DFF = 1024
DL_GROUPS = ((128, 1), (512, 4), (2048, 16))
SAME_ENG_SYNC = True
OWN0 = 48
ALIBI_TH = 44.0
DA_WT = [min(NT, int(math.ceil(ALIBI_TH / (128 * 2.0 ** (-(hh + 1))))) + 1) for hh in range(8)]


class T:
    __slots__ = ("name", "w", "rs")

    def __init__(self, name):
        self.name = name
        self.w = None
        self.rs = []


class Tl:
    def __init__(self, ap, name):
        self.ap = ap
        self.t = T(name)

    def __getitem__(self, k):
        return self.ap[k]


class Op:
    __slots__ = ("eng", "fn", "deps", "sig", "val", "isdma", "sem")


class Sched:
    ENG = ("pe", "act", "dve", "pool", "sp")

    def __init__(self, nc, es):
        self.nc = nc
        self.es = es
        self.e = {"pe": nc.tensor, "act": nc.scalar, "dve": nc.vector, "pool": nc.gpsimd, "sp": nc.sync}
        self.ops = []
        self.last = {k: None for k in self.ENG}
        self.sems = {}
        self.dcount = {}
        self.dma_since = {}
        self.pending = {k: [] for k in self.ENG}
        self.nsem = 0

    def tile(self, es, name, shape, dt):
        ap = es.enter_context(self.nc.sbuf_tensor("sb_" + name, list(shape), dt))
        return Tl(ap, name)

    def psum(self, es, name, shape, dt):
        ap = es.enter_context(self.nc.psum_tensor("ps_" + name, list(shape), dt))
        return Tl(ap, name)

    def dram(self, name):
        return T(name)

    def _sem(self, key):
        if key not in self.sems:
            self.sems[key] = self.es.enter_context(self.nc.semaphore(f"s{self.nsem}"))
            self.nsem += 1
        return self.sems[key]

    def add(self, eng, fn, kw, reads=(), writes=(), dma=None):
        op = Op()
        op.eng = eng
        op.fn = (fn, kw)
        op.sig = False
        op.val = 0
        op.isdma = dma is not None
        op.sem = None
        deps = []
        for r in reads:
            t = r.t if isinstance(r, Tl) else r
            if t.w is not None:
                deps.append(t.w)
        for w in writes:
            t = w.t if isinstance(w, Tl) else w
            if t.w is not None:
                deps.append(t.w)
            deps.extend(t.rs)
        deps.extend(self.pending[eng])
        self.pending[eng] = []
        op.deps = [d for d in dict.fromkeys(deps) if d is not op]
        for r in reads:
            t = r.t if isinstance(r, Tl) else r
            t.rs.append(op)
        for w in writes:
            t = w.t if isinstance(w, Tl) else w
            t.w = op
            t.rs = []
        if op.isdma:
            key = ("dma", dma)
            self._sem(key)
            self.dcount[key] = self.dcount.get(key, 0) + 16
            op.sem = key
            op.val = self.dcount[key]
            self.dma_since[key] = op
        else:
            self.last[eng] = op
        self.ops.append(op)
        return op

    def barrier(self):
        deps = [o for o in self.last.values() if o is not None] + list(self.dma_since.values())
        self.dma_since = {}
        for k in self.ENG:
            self.pending[k] = list(deps)

    def emit(self):
        for op in self.ops:
            for d in op.deps:
                if not d.isdma:
                    if d.eng == op.eng and (d.eng == "pe" or not SAME_ENG_SYNC):
                        continue
                    d.sig = True
        cnt = {k: 0 for k in self.ENG}
        for op in self.ops:
            if not op.isdma and op.sig:
                cnt[op.eng] += 1
                op.val = cnt[op.eng]
                op.sem = ("eng", op.eng)
                self._sem(op.sem)
        waited = {k: {} for k in self.ENG}
        nw = 0
        for op in self.ops:
            E = self.e[op.eng]
            need = {}
            for d in op.deps:
                if not d.isdma and d.eng == op.eng and (d.eng == "pe" or not SAME_ENG_SYNC):
                    continue
                if d.sem is None:
                    continue
                if d.val > need.get(d.sem, 0):
                    need[d.sem] = d.val
            for key, val in need.items():
                if waited[op.eng].get(key, 0) >= val:
                    continue
                waited[op.eng][key] = val
                E.wait_ge(self.sems[key], val)
                nw += 1
            try:
                ins = op.fn[0](**op.fn[1])
            except Exception:
                print("EMIT FAIL", op.eng, getattr(op.fn[0], "__name__", op.fn[0]), {k: (getattr(v, "shape", v), getattr(v, "dtype", None)) for k, v in op.fn[1].items()})
                raise
            if op.isdma:
                ins.then_inc(self.sems[op.sem], 16)
            elif op.sig:
                ins.then_inc(self.sems[op.sem], 1)
        fin = {}
        for op in self.ops:
            if op.sem is not None:
                fin[op.sem] = max(fin.get(op.sem, 0), op.val)
        for key, val in fin.items():
            if waited["sp"].get(key, 0) < val:
                self.nc.sync.wait_ge(self.sems[key], val)
        return len(self.ops), nw


def _bf16(a):
    return np.asarray(a, dtype=np.float32).astype(ml_dtypes.bfloat16)


def make_consts():
    c = {}
    c["ident_f"] = np.eye(128, dtype=np.float32)
    c["ident_b"] = _bf16(np.eye(128))
    kpos = np.arange(S_LEN)
    kb, kr = kpos // 128, kpos % 128
    kaug = np.zeros((8, 4, S_LEN), np.float32)
    for h in range(8):
        sl = 2.0 ** (-(h + 1))
        kaug[h, 0] = sl * 128 * kb
        kaug[h, 1] = sl * kr
        kaug[h, 2] = -sl * 128
        kaug[h, 3] = -sl
    c["kaug"] = _bf16(kaug)
    n = np.arange(NOWN * 128)
    qaug = np.zeros((4, NOWN * 128), np.float32)
    qaug[0] = 1.0
    qaug[1] = 1.0
    qaug[2] = OWN0 + n // 128
    qaug[3] = n % 128
    c["qaug"] = _bf16(qaug)
    dm = np.zeros((128, 4, 512), np.float32)
    ki = np.arange(128)[:, None]
    qi = np.arange(128)[None, :]
    for r in range(4):
        for u in range(4):
            if r > u:
                dm[:, r, u * 128:(u + 1) * 128] = -30000.0
            elif r == u:
                dm[:, r, u * 128:(u + 1) * 128] = np.where(ki <= qi, 0.0, -30000.0)
    c["dmask"] = _bf16(dm)
    tabs = []
    for g, (win, dil) in enumerate(DL_GROUPS):
        no = win // 128 + 1
        for o in range(no):
            dist = 128 * o + qi - ki
            ok = (dist >= 0) & (dist % dil == 0) & (dist <= win)
            tabs.append(np.where(ok, dist, 1e9).astype(np.float32))
    c["distm"] = np.ascontiguousarray(np.stack(tabs, axis=1))
    c["ltri"] = _bf16(np.triu(np.ones((128, 128)), k=1))
    c["ones_b"] = _bf16(np.ones((128, 128)))
    c["eoff"] = np.tile((np.arange(NEXP, dtype=np.float32) * CAP)[None, :], (128, 1))
    return c


DL_SLOPES = [2.0 ** (-8.0 * (h + 1) / 12) for h in range(12)]
DL_NOFF = [w // 128 + 1 for (w, d) in DL_GROUPS]
DL_TBASE = [0, 2, 7]


def build(debug=None, phases=(1, 2, 3, 4)):
    debug = debug or ()
    nc = bass.Bass("TRN2", target_bir_lowering=False)
    nc.dge_precook = False

    def din(name, shape, dt=F32):
        return nc.dram_tensor(name, list(shape), dt, kind="ExternalInput").ap()

    def dscr(name, shape, dt):
        kind = "ExternalOutput" if name in debug else "Internal"
        return nc.dram_tensor(name, list(shape), dt, kind=kind).ap()

    xs = din("xs", [NT * 128, D])
    valid_d = din("valid", [128, NT])
    w_in = din("w_in", [D, N_IN], F32R)
    nm_d = din("nm", [128, 16])
    nf_d = din("nf", [128, 16])
    gq_da_d = din("gq_da", [128, 64])
    gk_da_d = din("gk_da", [128, 64])
    gq_dl_d = din("gq_dl", [128, 128])
    gk_dl_d = din("gk_dl", [128, 128])
    subg_d = din("subg", [128, 128])
    lamq_d = din("lamq", [128, 128])
    lamk_d = din("lamk", [128, 128])
    w_a = din("w_a", [1024, D], F32R)
    w_b = din("w_b", [512, D], F32R)
    w_o = din("w_o", [D, D], F32R)
    wr_d = din("wr", [D, 36])
    if 4 in phases:
        w_gu = din("w_gu", [NEXP, D, 2 * DFF], F32R)
        w_dn = din("w_dn", [NEXP, DFF, D], F32R)
    ident_f_d = din("ident_f", [128, 128])
    ident_b_d = din("ident_b", [128, 128], BF16)
    kaug_d = din("kaug", [8, 4, S_LEN], BF16)
    qaug_d = din("qaug", [4, NOWN * 128], BF16)
    dmask_d = din("dmask", [128, 4, 512], BF16)
    distm_d = din("distm", [128, 24, 128])
    ltri_d = din("ltri", [128, 128], BF16)
    ones_d = din("ones_b", [128, 128], BF16)
    eoff_d = din("eoff", [128, NEXP])

    out_d = nc.dram_tensor("out", [NOWN * 128, D], F32, kind="ExternalOutput").ap()

    kT = dscr("kT", [16, 64, S_LEN], BF16)
    vS = dscr("vS", [8, NT, 128, 130], BF16)
    dkT = dscr("dkT", [12, 128, S_LEN], BF16)
    dvS = dscr("dvS", [12, NT, 128, 130], BF16)
    qT = dscr("qT", [16, 64, NOWN * 128], BF16)
    dqT = dscr("dqT", [12, 128, NOWN * 128], BF16)
    gS = dscr("gS", [NOWN, 128, 4096], F32)
    oA = dscr("oA", [NOWN, 128, 1024], F32)
    oB = dscr("oB", [NOWN, 128, 512], F32)
    mS = dscr("mS", [NOWN, 128, D], F32)
    xsd = dscr("xsd", [NEXP * CAP, D], F32)
    ysd = dscr("ysd", [NEXP * CAP, D], F32)

    es0 = ExitStack()
    with es0:
        S = Sched(nc, es0)
        es0.enter_context(nc.Block())
        E = S.e
        t_kT, t_vS, t_dkT, t_dvS, t_qT, t_dqT = (S.dram(n) for n in ("kT", "vS", "dkT", "dvS", "qT", "dqT"))
        t_gS, t_oA, t_oB, t_mS, t_xsd, t_ysd, t_out = (S.dram(n) for n in ("gS", "oA", "oB", "mS", "xsd", "ysd", "out"))

        def ptile(name, shape, dt=F32):
            return S.tile(es0, name, shape, dt)

        ident_f = ptile("ident_f", [128, 128])
        ident_b = ptile("ident_b", [128, 128], BF16)
        nm = ptile("nm", [128, 16])
        nf = ptile("nf", [128, 16])
        valid = ptile("valid", [128, NT])
        rstd_all = ptile("rstd_all", [128, NT])
        gqk_da = ptile("gqk_da", [128, 64])
        gqk_dl = ptile("gqk_dl", [128, 128])
        subg = ptile("subg", [128, 128])
        neglam = ptile("neglam", [128, 1])
        mhalf = ptile("mhalf", [128, 16])
        junk = ptile("junk", [128, 2048], BF16)
        jt = T("junk_untracked")

        V, ACT, PE, POOL = nc.vector, nc.scalar, nc.tensor, nc.gpsimd

        def A(eng, fn, reads, writes, **kw):
            S.add(eng, fn, kw, reads, writes)

        def DMA(key, out, in_, reads, writes, q="sp"):
            fn = {"sp": nc.sync.dma_start, "act": nc.scalar.dma_start, "pool": nc.gpsimd.dma_start}[q]
            S.add(q, fn, dict(out=out, in_=in_), reads, writes, dma=key)

        def ld(tl, src, key, out=None):
            DMA(key, tl.ap[:] if out is None else out, src, [], [tl])

        ld(ident_f, ident_f_d[:, :], "c0")
        ld(ident_b, ident_b_d[:, :], "c1")
        ld(nm, nm_d[:, :], "c2")
        ld(nf, nf_d[:, :], "c3")
        ld(valid, valid_d[:, :], "c4")
        ld(subg, subg_d[:, :], "c5")
        A("pool", POOL.memset, [], [mhalf], ap=mhalf.ap[:], constant=-0.5)
        with ExitStack() as es:
            ta = S.tile(es, "tmpa", [128, 128], F32)
            tb = S.tile(es, "tmpb", [128, 128], F32)
            e2 = S.tile(es, "tmpe", [128, 2], F32)
            ld(ta, gq_da_d[:, :], "c6", out=ta[:, 0:64])
            ld(tb, gk_da_d[:, :], "c7", out=tb[:, 0:64])
            A("dve", V.scalar_tensor_tensor, [ta, tb], [gqk_da], out=gqk_da.ap[:], in0=ta[:, 0:64], scalar=0.125, in1=tb[:, 0:64],
              op0=ALU.mult, op1=ALU.mult)
            ld(ta, gq_dl_d[:, :], "c6")
            ld(tb, gk_dl_d[:, :], "c7")
            A("dve", V.scalar_tensor_tensor, [ta, tb], [gqk_dl], out=gqk_dl.ap[:], in0=ta[:, :], scalar=128 ** -0.5, in1=tb[:, :],
              op0=ALU.mult, op1=ALU.mult)
            ld(ta, lamq_d[:, :], "c6")
            ld(tb, lamk_d[:, :], "c7")
            for i in range(2):
                A("dve", V.scalar_tensor_tensor, [ta, tb], [e2], out=junk[:, 0:64], in0=ta[:, i * 64:(i + 1) * 64], scalar=1.0,
                  in1=tb[:, i * 64:(i + 1) * 64], op0=ALU.mult, op1=ALU.mult, accum_out=e2[:, i:i + 1])
            A("act", ACT.activation, [e2], [e2], out=e2[:, :], in_=e2[:, :], func=AF.Exp)
            A("dve", V.scalar_tensor_tensor, [e2], [neglam], out=neglam.ap[:], in0=e2[:, 1:2], scalar=-0.2, in1=e2[:, 0:1],
              op0=ALU.add, op1=ALU.subtract)
            A("dve", V.tensor_scalar, [subg], [subg], out=subg.ap[:], in0=subg.ap[:], scalar1=0.8, scalar2=None, op0=ALU.mult)
            S.barrier()

        def rsqrt_ops(ss, rs_ap, n, inv, rs_tl):
            A("dve", V.tensor_scalar, [ss], [ss], out=ss[:, 0:n], in0=ss[:, 0:n], scalar1=inv, scalar2=EPS, op0=ALU.mult, op1=ALU.add)
            A("pool", POOL.tensor_tensor, [ss, mhalf], [rs_tl], out=rs_ap, in0=ss[:, 0:n], in1=mhalf[:, 0:n], op=ALU.pow)

        def sumsq(src_tl, src_ap, n, acc_tl, acc_ap):
            A("dve", V.scalar_tensor_tensor, [src_tl], [acc_tl], out=junk[:, 0:n], in0=src_ap, scalar=1.0, in1=src_ap,
              op0=ALU.mult, op1=ALU.mult, accum_out=acc_ap)

        if 1 in phases:
            with ExitStack() as es:
                TB = 8
                NB3 = 3
                xt = [S.tile(es, f"xt{i}", [128, D], F32) for i in range(2)]
                xT = [S.tile(es, f"xT{i}", [128, 16, 128], F32R) for i in range(TB)]
                wb = [S.tile(es, f"wb{i}", [128, 16, 512], F32R) for i in range(2)]
                pr = [S.tile(es, f"pr{i}", [128, 512], F32) for i in range(NB3)]
                sq = [S.tile(es, f"sq{i}", [128, 512], F32) for i in range(NB3)]
                kn = [S.tile(es, f"kn{i}", [128, 512], BF16) for i in range(NB3)]
                kst = [S.tile(es, f"kst{i}", [128, 8, 128], BF16) for i in range(2)]
                vst = [S.tile(es, f"vst{i}", [128, 4, 130], BF16) for i in range(2)]
                ssq = [S.tile(es, f"ssq{i}", [128, 8], F32) for i in range(NB3)]
                rsq = [S.tile(es, f"rsq{i}", [128, 8], F32) for i in range(NB3)]
                xss = [S.tile(es, f"xss{i}", [128, 1], F32) for i in range(2)]
                ps_mm = [S.psum(es, f"pmm{i}", [128, 512], F32) for i in range(2)]
                ps_tx = [S.psum(es, f"ptx{i}", [128, 4, 128], F32) for i in range(2)]
                ps_tk = [S.psum(es, f"ptk{i}", [128, 8, 128], BF16) for i in range(2)]
                rstd_t = [T(f"rstd{p}") for p in range(NT)]
                for v in vst:
                    A("pool", POOL.memset, [], [v], ap=v.ap[:], constant=0.0)
                ctr = {"x": 0, "w": 0, "mm": 0, "tx": 0, "pp": 0, "tk": 0, "v": 0}

                xseq = [(p, True) for p in range(NT)]
                xbuf = {}

                def issue_x(i):
                    if i >= len(xseq):
                        return
                    xi = xt[i % 2]
                    xbuf[i] = xi
                    p = xseq[i][0]
                    ld(xi, xs[p * 128:(p + 1) * 128, :], "x" + xi.t.name)

                def do_xT(i, slot):
                    p, need_rstd = xseq[i]
                    xi = xbuf[i]
                    if need_rstd:
                        sx = xss[i % 2]
                        sumsq(xi, xi[:, :], D, sx, sx[:, 0:1])
                        rsqrt_ops(sx, rstd_all[:, p:p + 1], 1, 1.0 / D, rstd_t[p])
                    for q in range(4):
                        bank = ps_tx[ctr["tx"] % 2]
                        ctr["tx"] += 1
                        for k in range(4):
                            c = 4 * q + k
                            A("pe", PE.transpose, [xi, ident_f], [bank], out=bank[:, k, :], in_=xi[:, c * 128:(c + 1) * 128], identity=ident_f[:, :])
                        A("dve", V.tensor_tensor, [bank, nm], [xT[slot]], out=xT[slot][:, 4 * q:4 * q + 4, :], in0=bank[:, :, :],
                          in1=nm[:, 4 * q:4 * q + 4].unsqueeze(2).to_broadcast([128, 4, 128]), op=ALU.mult)
                    issue_x(i + 2)

                def mm(slot, w):
                    acc = ps_mm[ctr["mm"] % 2]
                    ctr["mm"] += 1
                    for c in range(16):
                        A("pe", PE.matmul, [xT[slot], w], [acc], out=acc[:, :], lhsT=xT[slot][:, c, :], rhs=w[:, c, :],
                          start=(c == 0), stop=(c == 15))
                    return acc

                def post_norm(acc, p, G, W, gain, dst, dst_t, tok0):
                    i = ctr["pp"] % NB3
                    ctr["pp"] += 1
                    prt, sqt, knt, sst, rst = pr[i], sq[i], kn[i], ssq[i], rsq[i]
                    A("act", ACT.activation, [acc, rstd_t[p]], [prt], out=prt[:, :], in_=acc[:, :], func=AF.Copy, scale=rstd_all[:, p:p + 1])
                    A("pool", POOL.tensor_tensor, [prt], [sqt], out=sqt[:, :], in0=prt[:, :], in1=prt[:, :], op=ALU.mult)
                    A("dve", V.tensor_reduce, [sqt], [sst], out=sst[:, 0:G], in_=sqt[:, :].rearrange("p (g w) -> p g w", g=G), axis=AX.X, op=ALU.add)
                    rsqrt_ops(sst, rst[:, 0:G], G, 1.0 / W, rst)
                    pv = prt[:, :].rearrange("p (g w) -> p g w", g=G)
                    kv = knt[:, :].rearrange("p (g w) -> p g w", g=G)
                    rb = rst[:, 0:G].unsqueeze(2).to_broadcast([128, G, W])
                    if gain is None:
                        A("dve", V.tensor_tensor, [prt, rst], [knt], out=kv, in0=pv, in1=rb, op=ALU.mult)
                    else:
                        A("dve", V.tensor_tensor, [prt, rst], [prt], out=pv, in0=pv, in1=rb, op=ALU.mult)
                        A("pool", POOL.tensor_tensor, [prt, gain], [knt], out=kv, in0=pv,
                          in1=gain[:, 0:W].unsqueeze(1).to_broadcast([128, G, W]), op=ALU.mult)

                    def part_b():
                        j = ctr["tk"] % 2
                        ctr["tk"] += 1
                        kstt, ptk = kst[j], ps_tk[j]
                        for g in range(G):
                            A("pe", PE.transpose, [knt, ident_b], [ptk], out=ptk[0:W, g, :], in_=knt[:, g * W:(g + 1) * W], identity=ident_b[:, :])
                        A("act", ACT.copy, [ptk], [kstt], out=kstt[0:W, 0:G, :], in_=ptk[0:W, 0:G, :])
                        DMA("st" + kstt.t.name, dst[:, :, tok0:tok0 + 128].rearrange("h d t -> d h t"), kstt[0:W, 0:G, :], [kstt], [dst_t], q="act")
                    return part_b

                def post_v(acc, p, dst, dst_t):
                    v = vst[ctr["v"] % 2]
                    ctr["v"] += 1
                    A("act", ACT.activation, [acc, rstd_t[p]], [v], out=v[:, :, 0:128], in_=acc[:, :].rearrange("p (h c) -> p h c", h=4),
                      func=AF.Copy, scale=rstd_all[:, p:p + 1])
                    A("pool", POOL.tensor_copy, [valid], [v], out=v[:, :, 128:129], in_=valid[:, p:p + 1].unsqueeze(1).to_broadcast([128, 4, 1]))
                    DMA("st" + v.t.name, dst[:, p, :, :].rearrange("h p c -> p h c"), v[:, :, :], [v], [dst_t], q="act")
                    return None

                def post_gate(acc, p, m, gb):
                    i = ctr["pp"] % NB3
                    ctr["pp"] += 1
                    prt = pr[i]
                    A("act", ACT.activation, [acc, rstd_t[p]], [prt], out=prt[:, :], in_=acc[:, :], func=AF.Copy, scale=rstd_all[:, p:p + 1])
                    DMA("st" + prt.t.name, gS[m, :, gb * 512:(gb + 1) * 512], prt[:, :], [prt], [t_gS], q="act")
                    return None

                kv_blocks = ([("dak", b_) for b_ in range(2)] + [("dav", b_) for b_ in range(2)] + [("dlk", g) for g in range(3)]
                             + [("dlv", g) for g in range(3)])
                q_blocks = [("daq", b_) for b_ in range(2)] + [("dlq", g) for g in range(3)] + [("gate", g) for g in range(8)]
                col_of = {"dak": C_DAK, "dav": C_DAV, "dlk": C_DLK, "dlv": C_DLV, "daq": C_DAQ, "dlq": C_DLQ, "gate": C_GA}

                def first_tile(kind, bi):
                    if kind in ("dak", "dav"):
                        return max(0, OWN0 - max(DA_WT[4 * bi:4 * bi + 4]))
                    if kind in ("dlk", "dlv"):
                        return OWN0 - (DL_NOFF[bi] - 1)
                    return OWN0
                sbs = []
                for sb in range(NT // TB):
                    tiles = [sb * TB + tl for tl in range(TB)]
                    blocks = [(k_, b_) for (k_, b_) in kv_blocks + q_blocks if first_tile(k_, b_) <= tiles[-1]]
                    sbs.append((tiles, blocks))
                wseq = [(si, kind, bi) for si, (tiles, blocks) in enumerate(sbs) for (kind, bi) in blocks]
                wtile = {}

                def issue_w(i):
                    if i >= len(wseq):
                        return
                    w = wb[i % 2]
                    wtile[i] = w
                    col0 = col_of[wseq[i][1]] + 512 * wseq[i][2]
                    ld(w, w_in[:, col0:col0 + 512].rearrange("(c p) n -> p c n", p=128), "w" + w.t.name)

                pend = []
                issue_x(0)
                issue_x(1)
                issue_w(0)
                for tl in range(TB):
                    do_xT(tl, tl)
                wi = 0
                for si, (tiles, blocks) in enumerate(sbs):
                    for bk, (kind, bi) in enumerate(blocks):
                        w = wtile[wi]
                        issue_w(wi + 1)
                        wi += 1
                        ft = first_tile(kind, bi)
                        for tl, p in enumerate(tiles):
                            m = p - OWN0
                            if p >= ft:
                                acc = mm(tl, w)
                                if len(pend) >= 2:
                                    pb = pend.pop(0)
                                    if pb is not None:
                                        pb()
                                if kind == "dak":
                                    pb = post_norm(acc, p, 8, 64, None, kT[8 * bi:8 * bi + 8], t_kT, p * 128)
                                elif kind == "dlk":
                                    pb = post_norm(acc, p, 4, 128, None, dkT[4 * bi:4 * bi + 4], t_dkT, p * 128)
                                elif kind == "dav":
                                    pb = post_v(acc, p, vS[4 * bi:4 * bi + 4], t_vS)
                                elif kind == "dlv":
                                    pb = post_v(acc, p, dvS[4 * bi:4 * bi + 4], t_dvS)
                                elif kind == "daq":
                                    pb = post_norm(acc, p, 8, 64, gqk_da, qT[8 * bi:8 * bi + 8], t_qT, m * 128)
                                elif kind == "dlq":
                                    pb = post_norm(acc, p, 4, 128, gqk_dl, dqT[4 * bi:4 * bi + 4], t_dqT, m * 128)
                                else:
                                    pb = post_gate(acc, p, m, bi)
                                pend.append(pb)
                            if bk == len(blocks) - 1 and si + 1 < len(sbs):
                                do_xT((si + 1) * TB + tl, tl)
                for pb in pend:
                    if pb is not None:
                        pb()
                S.barrier()

        if 2 in phases:
            with ExitStack() as es:
                kth = [S.tile(es, f"kth{i}", [68, 2, S_LEN], BF16) for i in range(2)]
                qth = [S.tile(es, f"qth{i}", [68, 2, NOWN * 128], BF16) for i in range(2)]
                vh = [S.tile(es, f"vh{i}", [128, NT, 130], BF16) for i in range(2)]
                mk = S.tile(es, "mk", [128, 4, 512], BF16)
                pt = [S.tile(es, f"pt{k}", [128, 2, 512], BF16) for k in range(2)]
                oah = [S.tile(es, f"oah{i}", [128, NOWN, 128], F32) for i in range(2)]
                fr = [S.tile(es, f"fr{i}", [128, 4], F32) for i in range(2)]
                ft = [S.tile(es, f"ft{i}", [128, 128], F32) for i in range(2)]
                fo = [S.tile(es, f"fo{i}", [128, 128], F32) for i in range(2)]
                ps_s = [S.psum(es, f"pss{k}", [128, 2, 512], F32) for k in range(2)]
                ps_o = [S.psum(es, f"pso{k}", [128, 3, 130], F32) for k in range(3)]
                ld(mk, dmask_d[:, :, :], "mk")

                def load_head(h):
                    hb = h % 2
                    c0 = max(0, OWN0 - DA_WT[h]) * 128
                    t0 = c0 // 128
                    for i in range(2):
                        DMA(f"kth{hb}", kth[hb][0:64, i, c0:], kT[2 * h + i, :, c0:], [t_kT], [kth[hb]])
                        DMA(f"kth{hb}", kth[hb][64:68, i, c0:], kaug_d[h, :, c0:], [], [kth[hb]])
                        DMA(f"qth{hb}", qth[hb][0:64, i, :], qT[2 * h + i, :, :], [t_qT], [qth[hb]])
                        DMA(f"qth{hb}", qth[hb][64:68, i, :], qaug_d[:, :], [], [qth[hb]])
                    DMA(f"vh{hb}", vh[hb][:, t0:, :], vS[h, t0:].rearrange("t p c -> p t c"), [t_vS], [vh[hb]])

                load_head(0)
                fin = 0
                for h in range(8):
                    hb = h % 2
                    if h + 1 < 8:
                        load_head(h + 1)
                    for a in range(4):
                        d0 = OWN0 + 4 * a
                        lo = max(0, d0 - DA_WT[h])
                        kts = list(range(lo, d0 + 4))

                        def qk(kt):
                            for i in range(2):
                                s_ = ps_s[kt % 2]
                                diag = kt >= d0
                                A("pe", PE.matmul, [kth[hb], qth[hb]], [s_], out=s_[:, i, :], lhsT=kth[hb][0:68, i, kt * 128:(kt + 1) * 128],
                                  rhs=qth[hb][0:68, i, a * 512:(a + 1) * 512], start=True, stop=not diag)
                                if diag:
                                    A("pe", PE.matmul, [ident_b, mk], [s_], out=s_[:, i, :], lhsT=ident_b[:, :], rhs=mk[:, kt - d0, :],
                                      start=False, stop=True)

                        qk(kts[0])
                        for kt in kts:
                            if kt + 1 <= kts[-1]:
                                qk(kt + 1)
                            A("act", ACT.activation, [ps_s[kt % 2]], [pt[kt % 2]], out=pt[kt % 2][:, :, :], in_=ps_s[kt % 2][:, :, :], func=AF.Exp)
                            for i in range(2):
                                for u in range(4):
                                    last = d0 + u
                                    if kt > last:
                                        continue
                                    idx = i * 4 + u
                                    bank = ps_o[idx // 3]
                                    A("pe", PE.matmul, [pt[kt % 2], vh[hb]], [bank], out=bank[:, idx % 3, :],
                                      lhsT=pt[kt % 2][:, i, u * 128:(u + 1) * 128], rhs=vh[hb][:, kt, :],
                                      start=(kt == lo and idx % 3 == 0), stop=(kt == last), skip_group_check=True)
                        for u in range(4):
                            m = 4 * a + u
                            f = fin % 2
                            fin += 1
                            b0, b1 = ps_o[u // 3], ps_o[(4 + u) // 3]
                            a0, a1 = b0[:, u % 3, :], b1[:, (4 + u) % 3, :]
                            A("dve", V.reciprocal, [b0], [fr[f]], out=fr[f][:, 0:1], in_=a0[:, 128:129])
                            A("dve", V.reciprocal, [b1], [fr[f]], out=fr[f][:, 1:2], in_=a1[:, 128:129])
                            A("dve", V.tensor_scalar, [b1, fr[f], neglam], [ft[f]], out=ft[f][:, :], in0=a1[:, 0:128], scalar1=fr[f][:, 1:2],
                              scalar2=neglam[:, 0:1], op0=ALU.mult, op1=ALU.mult)
                            A("dve", V.scalar_tensor_tensor, [b0, fr[f], ft[f]], [fo[f]], out=fo[f][:, :], in0=a0[:, 0:128], scalar=fr[f][:, 0:1],
                              in1=ft[f][:, :], op0=ALU.mult, op1=ALU.add)
                            sumsq(fo[f], fo[f][:, :], 128, fr[f], fr[f][:, 2:3])
                            A("dve", V.tensor_scalar, [fr[f]], [fr[f]], out=fr[f][:, 2:3], in0=fr[f][:, 2:3], scalar1=1.0 / 128, scalar2=EPS,
                              op0=ALU.mult, op1=ALU.add)
                            A("pool", POOL.tensor_tensor, [fr[f], mhalf], [fr[f]], out=fr[f][:, 3:4], in0=fr[f][:, 2:3], in1=mhalf[:, 0:1], op=ALU.pow)
                            A("dve", V.scalar_tensor_tensor, [fo[f], fr[f], subg], [oah[hb]], out=oah[hb][:, m, :], in0=fo[f][:, :],
                              scalar=fr[f][:, 3:4], in1=subg[:, :], op0=ALU.mult, op1=ALU.mult)
                    DMA(f"oah{hb}", oA[:, :, h * 128:(h + 1) * 128].rearrange("m p c -> p m c"), oah[hb][:, :, :], [oah[hb]], [t_oA])
                S.barrier()

            with ExitStack() as es:
                NR = 4
                kd = [S.tile(es, f"kd{g}", [128, S_LEN], BF16) for g in range(3)]
                vd = [S.tile(es, f"vd{g}", [128, NT, 130], BF16) for g in range(3)]
                qd = [S.tile(es, f"qd{g}", [128, NOWN * 128], BF16) for g in range(3)]
                dist = S.tile(es, "dist", [128, 24, 128], F32)
                sc = [S.tile(es, f"sc{i}", [128, 512], F32) for i in range(NR)]
                ptd = [S.tile(es, f"ptd{i}", [128, 512], BF16) for i in range(NR)]
                obh = [S.tile(es, f"obh{i}", [128, NOWN, 128], F32) for i in range(2)]
                frd = [S.tile(es, f"frd{i}", [128, 1], F32) for i in range(4)]
                ps_sd = [S.psum(es, f"psd{i}", [128, 512], F32) for i in range(NR)]
                ps_od = [S.psum(es, f"pod{i}", [128, 130], F32) for i in range(4)]
                ld(dist, distm_d[:, :, :], "dist")
                for h in range(4):
                    hb = h % 2
                    for g in range(3):
                        gh = g * 4 + h
                        DMA(f"kd{g}", kd[g][:, 32 * 128:], dkT[gh, :, 32 * 128:], [t_dkT], [kd[g]])
                        DMA(f"vd{g}", vd[g][:, 32:, :], dvS[gh, 32:].rearrange("t p c -> p t c"), [t_dvS], [vd[g]])
                        DMA(f"qd{g}", qd[g][:, :], dqT[gh, :, :], [t_dqT], [qd[g]])
                    for qg in range(4):
                        p0 = OWN0 + 4 * qg
                        steps = []
                        for g in range(3):
                            no = DL_NOFF[g]
                            for kp in range(p0 - (no - 1), p0 + 4):
                                ulo, uhi = max(0, kp - p0), min(3, kp - p0 + no - 1)
                                steps.append((g, kp, ulo, uhi))
                        first_u, last_u = {}, {}
                        for si_, (g, kp, ulo, uhi) in enumerate(steps):
                            for u in range(ulo, uhi + 1):
                                first_u.setdefault(u, si_)
                                last_u[u] = si_

                        def dqk(si_):
                            g, kp, ulo, uhi = steps[si_]
                            s_ = ps_sd[si_ % NR]
                            A("pe", PE.matmul, [kd[g], qd[g]], [s_], out=s_[:, ulo * 128:(uhi + 1) * 128], lhsT=kd[g][:, kp * 128:(kp + 1) * 128],
                              rhs=qd[g][:, (4 * qg + ulo) * 128:(4 * qg + uhi + 1) * 128], start=True, stop=True)

                        dqk(0)
                        dqk(1)
                        dqk(2)
                        for si_, (g, kp, ulo, uhi) in enumerate(steps):
                            if si_ + 3 < len(steps):
                                dqk(si_ + 3)
                            r_ = si_ % NR
                            nu = uhi - ulo + 1
                            o0 = p0 + ulo - kp
                            A("dve", V.scalar_tensor_tensor, [dist, ps_sd[r_]], [sc[r_]], out=sc[r_][:, ulo * 128:(uhi + 1) * 128].rearrange("p (u c) -> p u c", u=nu),
                              in0=dist[:, DL_TBASE[g] + o0:DL_TBASE[g] + o0 + nu, :], scalar=-DL_SLOPES[g * 4 + h],
                              in1=ps_sd[r_][:, ulo * 128:(uhi + 1) * 128].rearrange("p (u c) -> p u c", u=nu), op0=ALU.mult, op1=ALU.add)
                            A("act", ACT.activation, [sc[r_]], [ptd[r_]], out=ptd[r_][:, ulo * 128:(uhi + 1) * 128], in_=sc[r_][:, ulo * 128:(uhi + 1) * 128],
                              func=AF.Exp)
                            for u in range(ulo, uhi + 1):
                                A("pe", PE.matmul, [ptd[r_], vd[g]], [ps_od[u]], out=ps_od[u][:, :], lhsT=ptd[r_][:, u * 128:(u + 1) * 128], rhs=vd[g][:, kp, :],
                                  start=(first_u[u] == si_), stop=(last_u[u] == si_))
                        for u in range(4):
                            m = 4 * qg + u
                            A("dve", V.reciprocal, [ps_od[u]], [frd[u]], out=frd[u][:, 0:1], in_=ps_od[u][:, 128:129])
                            A("dve", V.tensor_scalar, [ps_od[u], frd[u]], [obh[hb]], out=obh[hb][:, m, :], in0=ps_od[u][:, 0:128], scalar1=frd[u][:, 0:1],
                              scalar2=None, op0=ALU.mult)
                    DMA(f"obh{hb}", oB[:, :, h * 128:(h + 1) * 128].rearrange("m p c -> p m c"), obh[hb][:, :, :], [obh[hb]], [t_oB])
                S.barrier()

        if 3 in phases:
            with ExitStack() as es:
                wa = S.tile(es, "wa", [128, 8, D], F32R)
                wbb = S.tile(es, "wbb", [128, 4, D], F32R)
                ot = [S.tile(es, f"ot{i}", [128, 1536], F32) for i in range(2)]
                oT = [S.tile(es, f"oT{i}", [128, 12, 128], F32R) for i in range(2)]
                gt = [S.tile(es, f"gt{i}", [128, 4096], F32) for i in range(2)]
                mx = [S.tile(es, f"mx{i}", [128, D], F32) for i in range(2)]
                t1 = [S.tile(es, f"t1{i}", [128, 512], F32) for i in range(2)]
                t2 = [S.tile(es, f"t2{i}", [128, 512], F32) for i in range(2)]
                ps_t = [S.psum(es, f"p3t{i}", [128, 4, 128], F32) for i in range(2)]
                ps_a = [S.psum(es, f"p3a{i}", [128, 512], F32) for i in range(2)]
                ps_b = [S.psum(es, f"p3b{i}", [128, 512], F32) for i in range(2)]

                def load3(m):
                    i = m % 2
                    DMA(f"ot{i}", ot[i][:, 0:1024], oA[m, :, :], [t_oA], [ot[i]])
                    DMA(f"ot{i}", ot[i][:, 1024:1536], oB[m, :, :], [t_oB], [ot[i]])
                    DMA(f"gt{i}", gt[i][:, :], gS[m, :, :], [t_gS], [gt[i]])

                def prep3(m):
                    i = m % 2
                    for q in range(3):
                        bank = ps_t[q % 2]
                        for k in range(4):
                            c = 4 * q + k
                            A("pe", PE.transpose, [ot[i], ident_f], [bank], out=bank[:, k, :], in_=ot[i][:, c * 128:(c + 1) * 128], identity=ident_f[:, :])
                        A("act", ACT.copy, [bank], [oT[i]], out=oT[i][:, 4 * q:4 * q + 4, :], in_=bank[:, :, :])
                    A("act", ACT.activation, [gt[i]], [gt[i]], out=gt[i][:, :], in_=gt[i][:, :], func=AF.Sigmoid)

                load3(0)
                load3(1)
                DMA("wa", wa[:, :, :], w_a.rearrange("(c p) n -> p c n", p=128), [], [wa])
                DMA("wbb", wbb[:, :, :], w_b.rearrange("(c p) n -> p c n", p=128), [], [wbb])
                prep3(0)
                k3 = 0
                for m in range(NOWN):
                    i = m % 2
                    if m + 1 < NOWN:
                        prep3(m + 1)
                    for cb in range(4):
                        pa, pb = ps_a[k3 % 2], ps_b[k3 % 2]
                        ta_, tb_ = t1[k3 % 2], t2[k3 % 2]
                        k3 += 1
                        for c in range(8):
                            A("pe", PE.matmul, [oT[i], wa], [pa], out=pa[:, :], lhsT=oT[i][:, c, :], rhs=wa[:, c, cb * 512:(cb + 1) * 512],
                              start=(c == 0), stop=(c == 7))
                        for c in range(4):
                            A("pe", PE.matmul, [oT[i], wbb], [pb], out=pb[:, :], lhsT=oT[i][:, 8 + c, :], rhs=wbb[:, c, cb * 512:(cb + 1) * 512],
                              start=(c == 0), stop=(c == 3))
                        A("dve", V.tensor_tensor, [pa, gt[i]], [ta_], out=ta_[:, :], in0=pa[:, :], in1=gt[i][:, cb * 512:(cb + 1) * 512], op=ALU.mult)
                        A("dve", V.tensor_tensor, [pb, gt[i]], [tb_], out=tb_[:, :], in0=pb[:, :], in1=gt[i][:, 2048 + cb * 512:2048 + (cb + 1) * 512],
                          op=ALU.mult)
                        A("pool", POOL.tensor_tensor, [ta_, tb_], [mx[i]], out=mx[i][:, cb * 512:(cb + 1) * 512], in0=ta_[:, :], in1=tb_[:, :], op=ALU.add)
                    DMA(f"mx{i}", mS[m, :, :], mx[i][:, :], [mx[i]], [t_mS], q="act")
                    if m + 2 < NOWN:
                        load3(m + 2)
                S.barrier()

            with ExitStack() as es:
                TB = 8
                mt = [S.tile(es, f"mt{i}", [128, D], F32) for i in range(2)]
                mT = [S.tile(es, f"mT{i}", [128, 16, 128], F32R) for i in range(TB)]
                wo = [S.tile(es, f"wo{i}", [128, 16, 512], F32R) for i in range(2)]
                xo = [S.tile(es, f"xo{i}", [128, 512], F32) for i in range(3)]
                rs_ = [S.tile(es, f"rs{i}", [128, 512], F32) for i in range(2)]
                ps_t = [S.psum(es, f"p4t{i}", [128, 4, 128], F32) for i in range(2)]
                ps_a = [S.psum(es, f"p4a{i}", [128, 512], F32) for i in range(2)]
                k3 = 0
                wlist = [(sb, cb) for sb in range(NOWN // TB) for cb in range(4)]
                wot = {}

                def issue_wo(i):
                    if i >= len(wlist):
                        return
                    w = wo[i % 2]
                    wot[i] = w
                    cb = wlist[i][1]
                    DMA("wo" + w.t.name, w[:, :, :], w_o[:, cb * 512:(cb + 1) * 512].rearrange("(c p) n -> p c n", p=128), [], [w])

                def load_mt(m):
                    if m < NOWN:
                        DMA(f"mt{m % 2}", mt[m % 2][:, :], mS[m, :, :], [t_mS], [mt[m % 2]])

                issue_wo(0)
                load_mt(0)
                load_mt(1)
                wi = 0
                for sb in range(NOWN // TB):
                    for tl in range(TB):
                        m = sb * TB + tl
                        i = m % 2
                        for q in range(4):
                            bank = ps_t[q % 2]
                            for k in range(4):
                                c = 4 * q + k
                                A("pe", PE.transpose, [mt[i], ident_f], [bank], out=bank[:, k, :], in_=mt[i][:, c * 128:(c + 1) * 128], identity=ident_f[:, :])
                            A("act", ACT.copy, [bank], [mT[tl]], out=mT[tl][:, 4 * q:4 * q + 4, :], in_=bank[:, :, :])
                        load_mt(m + 2)
                    for cb in range(4):
                        w = wot[wi]
                        issue_wo(wi + 1)
                        wi += 1
                        for tl in range(TB):
                            m = sb * TB + tl
                            p = OWN0 + m
                            acc, xo_, r_ = ps_a[k3 % 2], xo[k3 % 3], rs_[k3 % 2]
                            k3 += 1
                            DMA("xo" + xo_.t.name, xo_[:, :], xs[p * 128:(p + 1) * 128, cb * 512:(cb + 1) * 512], [], [xo_])
                            for c in range(16):
                                A("pe", PE.matmul, [mT[tl], w], [acc], out=acc[:, :], lhsT=mT[tl][:, c, :], rhs=w[:, c, :], start=(c == 0), stop=(c == 15))
                            A("dve", V.tensor_tensor, [acc, xo_], [r_], out=r_[:, :], in0=acc[:, :], in1=xo_[:, :], op=ALU.add)
                            DMA("rs" + r_.t.name, out_d[m * 128:(m + 1) * 128, cb * 512:(cb + 1) * 512], r_[:, :], [r_], [t_out], q="act")
                S.barrier()

        if 4 in phases:
            dsti = ptile("dsti", [128, NOWN, 2], I32)
            wall = ptile("wall", [128, NOWN, 2], F32)
            bc_reg = nc.gpsimd.alloc_register("bc")
            S.add("pool", POOL.reg_mov, dict(out_reg=bc_reg, val=NEXP * CAP - 1), [], [])
            with ExitStack() as es:
                NIL = 4
                x2 = [S.tile(es, f"x2{i}", [128, D], F32) for i in range(NIL)]
                xn = [S.tile(es, f"xn{i}", [128, D], F32) for i in range(NIL)]
                xnT = [S.tile(es, f"xnT{i}", [128, 16, 128], F32) for i in range(NIL)]
                wr = S.tile(es, "wr", [128, 16, 36], F32)
                ltri = S.tile(es, "ltri", [128, 128], BF16)
                onesb = S.tile(es, "onesb", [128, 128], BF16)
                eoff = S.tile(es, "eoff", [128, NEXP], F32)
                tot = S.tile(es, "tot", [128, NEXP], F32)
                sm = [S.tile(es, f"sm{i}", [128, 16], F32) for i in range(NIL)]
                lg = [S.tile(es, f"lg{i}", [128, 36], F32) for i in range(NIL)]
                ge = [S.tile(es, f"ge{i}", [128, 4], F32) for i in range(NIL)]
                gm = [S.tile(es, f"gm{i}", [128, 4], F32) for i in range(NIL)]
                elm = [S.tile(es, f"elm{i}", [128, NEXP], F32) for i in range(NIL)]
                mx8 = [S.tile(es, f"mx8{i}", [128, 8], F32) for i in range(NIL)]
                M1 = [S.tile(es, f"M1{i}", [128, NEXP], F32) for i in range(NIL)]
                M2 = [S.tile(es, f"M2{i}", [128, NEXP], F32) for i in range(NIL)]
                Mb = [S.tile(es, f"Mb{i}", [128, NEXP], BF16) for i in range(NIL)]
                slot = [S.tile(es, f"slot{i}", [128, NEXP], F32) for i in range(NIL)]
                ovf = [S.tile(es, f"ovf{i}", [128, NEXP], F32) for i in range(NIL)]
                ps_t = [S.psum(es, f"p5t{i}", [128, 4, 128], F32) for i in range(4)]
                ps_l = [S.psum(es, f"p5l{i}", [128, 36], F32) for i in range(2)]
                ps_r = [S.psum(es, f"p5r{i}", [128, 2, NEXP], F32) for i in range(2)]
                ld(wr, wr_d.rearrange("(c p) n -> p c n", p=128), "wr")
                ld(ltri, ltri_d[:, :], "ltri")
                ld(onesb, ones_d[:, :], "onesb")
                ld(eoff, eoff_d[:, :], "eoff")
                A("dve", V.memset, [], [tot], ap=tot.ap[:], constant=0.0)

                def route_tile(m):
                    i = m % NIL
                    s_ = sm[i]
                    pl, prk = ps_l[m % 2], ps_r[m % 2]
                    DMA(f"x2{i}", x2[i][:, :], out_d[m * 128:(m + 1) * 128, :], [t_out], [x2[i]])
                    sumsq(x2[i], x2[i][:, :], D, s_, s_[:, 0:1])
                    yield
                    A("dve", V.tensor_scalar, [s_], [s_], out=s_[:, 0:1], in0=s_[:, 0:1], scalar1=1.0 / D, scalar2=EPS, op0=ALU.mult, op1=ALU.add)
                    A("pool", POOL.tensor_tensor, [s_, mhalf], [s_], out=s_[:, 1:2], in0=s_[:, 0:1], in1=mhalf[:, 0:1], op=ALU.pow)
                    A("act", ACT.activation, [x2[i], s_], [xn[i]], out=xn[i][:, :], in_=x2[i][:, :], func=AF.Copy, scale=s_[:, 1:2])
                    yield
                    for q in range(4):
                        bank = ps_t[q]
                        for k in range(4):
                            c = 4 * q + k
                            A("pe", PE.transpose, [xn[i], ident_f], [bank], out=bank[:, k, :], in_=xn[i][:, c * 128:(c + 1) * 128], identity=ident_f[:, :])
                        A("dve", V.tensor_tensor, [bank, nf], [xnT[i]], out=xnT[i][:, 4 * q:4 * q + 4, :], in0=bank[:, :, :],
                          in1=nf[:, 4 * q:4 * q + 4].unsqueeze(2).to_broadcast([128, 4, 128]), op=ALU.mult)
                        yield
                    for c in range(16):
                        A("pe", PE.matmul, [xnT[i], wr], [pl], out=pl[:, :], lhsT=xnT[i][:, c, :], rhs=wr[:, c, :], start=(c == 0), stop=(c == 15))
                    A("dve", V.tensor_copy, [pl], [lg[i]], out=lg[i][:, :], in_=pl[:, :])
                    yield
                    A("dve", V.tensor_reduce, [lg[i]], [s_], out=s_[:, 2:3], in_=lg[i][:, 0:4], axis=AX.X, op=ALU.max)
                    yield
                    A("dve", V.tensor_scalar, [s_], [s_], out=s_[:, 3:4], in0=s_[:, 2:3], scalar1=-1.0, scalar2=None, op0=ALU.mult)
                    yield
                    A("act", ACT.activation, [lg[i], s_], [ge[i], s_], out=ge[i][:, :], in_=lg[i][:, 0:4], func=AF.Exp, bias=s_[:, 3:4], accum_out=s_[:, 4:5])
                    A("dve", V.tensor_scalar, [lg[i], s_], [gm[i]], out=gm[i][:, :], in0=lg[i][:, 0:4], scalar1=s_[:, 2:3], scalar2=None, op0=ALU.is_equal)
                    yield
                    A("dve", V.tensor_scalar, [gm[i]], [gm[i]], out=gm[i][:, :], in0=gm[i][:, :], scalar1=-1.0, scalar2=1e30, op0=ALU.add, op1=ALU.mult)
                    yield
                    A("dve", V.tensor_tensor, [lg[i], gm[i]], [elm[i]], out=elm[i][:, :].rearrange("p (g e) -> p g e", g=4),
                      in0=lg[i][:, 4:36].rearrange("p (g e) -> p g e", g=4), in1=gm[i][:, 0:4].unsqueeze(2).to_broadcast([128, 4, 8]), op=ALU.add)
                    yield
                    A("dve", V.max, [elm[i]], [mx8[i]], out=mx8[i][:, :], in_=elm[i][:, :])
                    yield
                    A("dve", V.tensor_scalar, [elm[i], mx8[i]], [M1[i]], out=M1[i][:, :], in0=elm[i][:, :], scalar1=mx8[i][:, 0:1], scalar2=None, op0=ALU.is_equal)
                    A("dve", V.tensor_scalar, [elm[i], mx8[i]], [M2[i]], out=M2[i][:, :], in0=elm[i][:, :], scalar1=mx8[i][:, 1:2], scalar2=None, op0=ALU.is_equal)
                    A("dve", V.tensor_tensor, [mx8[i]], [s_], out=s_[:, 6:7], in0=mx8[i][:, 0:1], in1=mx8[i][:, 1:2], op=ALU.subtract)
                    yield
                    A("dve", V.tensor_tensor, [M1[i], M2[i]], [Mb[i]], out=Mb[i][:, :], in0=M1[i][:, :], in1=M2[i][:, :], op=ALU.add)
                    A("dve", V.reciprocal, [s_], [s_], out=s_[:, 5:6], in_=s_[:, 4:5])
                    A("act", ACT.activation, [s_], [s_], out=s_[:, 7:8], in_=s_[:, 6:7], func=AF.Sigmoid)
                    yield
                    A("pe", PE.matmul, [ltri, Mb[i]], [prk], out=prk[:, 0, :], lhsT=ltri[:, :], rhs=Mb[i][:, :], start=True, stop=True)
                    A("pe", PE.matmul, [onesb, Mb[i]], [prk], out=prk[:, 1, :], lhsT=onesb[:, :], rhs=Mb[i][:, :], start=False, stop=True, skip_group_check=True)
                    A("dve", V.tensor_tensor, [s_], [wall], out=wall[:, m, 0:1], in0=s_[:, 7:8], in1=s_[:, 5:6], op=ALU.mult)
                    yield
                    A("dve", V.tensor_tensor, [s_, wall], [wall], out=wall[:, m, 1:2], in0=s_[:, 5:6], in1=wall[:, m, 0:1], op=ALU.subtract)
                    A("dve", V.tensor_tensor, [prk, tot], [slot[i]], out=slot[i][:, :], in0=prk[:, 0, :], in1=tot[:, :], op=ALU.add)
                    A("dve", V.tensor_tensor, [prk, tot], [tot], out=tot[:, :], in0=prk[:, 1, :], in1=tot[:, :], op=ALU.add)
                    yield
                    A("dve", V.tensor_scalar, [slot[i]], [ovf[i]], out=ovf[i][:, :], in0=slot[i][:, :], scalar1=float(CAP) - 0.5, scalar2=1e6, op0=ALU.is_ge, op1=ALU.mult)
                    A("dve", V.tensor_tensor, [slot[i], eoff], [slot[i]], out=slot[i][:, :], in0=slot[i][:, :], in1=eoff[:, :], op=ALU.add)
                    yield
                    A("dve", V.tensor_tensor, [slot[i], ovf[i]], [slot[i]], out=slot[i][:, :], in0=slot[i][:, :], in1=ovf[i][:, :], op=ALU.add)
                    yield
                    for k, Mk in enumerate((M1[i], M2[i])):
                        A("dve", V.scalar_tensor_tensor, [Mk, slot[i]], [s_], out=junk[:, 0:NEXP], in0=Mk[:, :], scalar=1.0, in1=slot[i][:, :],
                          op0=ALU.mult, op1=ALU.mult, accum_out=s_[:, 8 + k:9 + k])
                    yield
                    for k in range(2):
                        A("dve", V.tensor_copy, [s_], [dsti], out=dsti[:, m, k:k + 1], in_=s_[:, 8 + k:9 + k])
                    yield
                    for k in range(2):
                        S.add("pool", POOL.indirect_dma_start,
                              dict(out=xsd[:, :], out_offset=bass.IndirectOffsetOnAxis(ap=dsti[:, m, k:k + 1], axis=0), in_=xn[i][:, :], in_offset=None,
                                   bounds_check=bc_reg, oob_is_err=False),
                              [xn[i], dsti], [t_xsd], dma=f"sc{i}")

                gens = [route_tile(m) for m in range(NOWN)]
                active = []
                nxt = 0
                step = 0
                while nxt < NOWN or active:
                    if nxt < NOWN and len(active) < NIL and step % 3 == 0:
                        active.append(gens[nxt])
                        nxt += 1
                    for gI in list(active):
                        try:
                            next(gI)
                        except StopIteration:
                            active.remove(gI)
                    step += 1
                S.barrier()

            with ExitStack() as es:
                xsb2 = [S.tile(es, f"xsb{i}", [128, D], F32) for i in range(2)]
                xsT = S.tile(es, "xsT", [128, 16, CAP], F32R)
                wgu = [S.tile(es, f"wgu{i}", [128, 8, DFF], F32R) for i in range(2)]
                wd = S.tile(es, "wd", [128, 8, D], F32R)
                sg = [S.tile(es, f"sg{i}", [128, CAP], F32) for i in range(8)]
                actT = S.tile(es, "actT", [128, 8, CAP], F32R)
                yb = [S.tile(es, f"yb{i}", [128, D], F32) for i in range(2)]
                ps_t = [S.psum(es, f"p6t{i}", [128, 4, 128], F32) for i in range(2)]
                ps_g = [S.psum(es, f"p6g{i}", [128, 2, CAP], F32) for i in range(4)]
                ps_d = [S.psum(es, f"p6d{i}", [128, 512], F32) for i in range(2)]
                kt_ = kw_ = 0
                for e in range(NEXP):
                    for blk in range(2):
                        xsb = xsb2[blk]
                        DMA(f"xsb{blk}", xsb[:, :], xsd[e * CAP + blk * 128:e * CAP + (blk + 1) * 128, :], [t_xsd], [xsb], q="pool")
                        for q in range(4):
                            bank = ps_t[kt_ % 2]
                            kt_ += 1
                            for k in range(4):
                                c = 4 * q + k
                                A("pe", PE.transpose, [xsb, ident_f], [bank], out=bank[:, k, :], in_=xsb[:, c * 128:(c + 1) * 128], identity=ident_f[:, :])
                            A("dve", V.tensor_tensor, [bank, nf], [xsT], out=xsT[:, 4 * q:4 * q + 4, blk * 128:(blk + 1) * 128], in0=bank[:, :, :],
                              in1=nf[:, 4 * q:4 * q + 4].unsqueeze(2).to_broadcast([128, 4, 128]), op=ALU.mult)
                    for part in range(2):
                        for dh in range(2):
                            w = wgu[kw_ % 2]
                            kw_ += 1
                            DMA("wgu" + w.t.name, w[:, :, :],
                                w_gu[e, dh * 1024:(dh + 1) * 1024, part * DFF:(part + 1) * DFF].rearrange("(c p) n -> p c n", p=128), [], [w])
                            for f in range(8):
                                pg = ps_g[f // 2]
                                for c in range(8):
                                    A("pe", PE.matmul, [w, xsT], [pg], out=pg[:, f % 2, :], lhsT=w[:, c, f * 128:(f + 1) * 128], rhs=xsT[:, dh * 8 + c, :],
                                      start=(dh == 0 and c == 0 and f % 2 == 0), stop=(dh == 1 and c == 7), skip_group_check=True)
                                if dh == 1:
                                    if part == 0:
                                        A("act", ACT.activation, [pg], [sg[f]], out=sg[f][:, :], in_=pg[:, f % 2, :], func=AF.Silu)
                                    else:
                                        A("dve", V.tensor_tensor, [sg[f], pg], [actT], out=actT[:, f, :], in0=sg[f][:, :], in1=pg[:, f % 2, :], op=ALU.mult)
                    DMA("wd", wd[:, :, :], w_dn[e, :, :].rearrange("(c p) n -> p c n", p=128), [], [wd])
                    for cb in range(4):
                        for blk in range(2):
                            acc = ps_d[(cb * 2 + blk) % 2]
                            for f in range(8):
                                A("pe", PE.matmul, [actT, wd], [acc], out=acc[:, :], lhsT=actT[:, f, blk * 128:(blk + 1) * 128], rhs=wd[:, f, cb * 512:(cb + 1) * 512],
                                  start=(f == 0), stop=(f == 7))
                            A("act", ACT.copy, [acc], [yb[blk]], out=yb[blk][:, cb * 512:(cb + 1) * 512], in_=acc[:, :])
                    for blk in range(2):
                        DMA(f"yb{blk}", ysd[e * CAP + blk * 128:e * CAP + (blk + 1) * 128, :], yb[blk][:, :], [yb[blk]], [t_ysd], q="act")
                S.barrier()

            with ExitStack() as es:
                NC4 = 4
                x2 = [S.tile(es, f"x2c{i}", [128, D], F32) for i in range(NC4)]
                y1 = [S.tile(es, f"y1{i}", [128, D], F32) for i in range(NC4)]
                y2 = [S.tile(es, f"y2{i}", [128, D], F32) for i in range(NC4)]

                def load4c(m):
                    if m >= NOWN:
                        return
                    i = m % NC4
                    DMA(f"x2c{i}", x2[i][:, :], out_d[m * 128:(m + 1) * 128, :], [t_out], [x2[i]])
                    for k, yk in enumerate((y1[i], y2[i])):
                        S.add("pool", POOL.indirect_dma_start,
                              dict(out=yk[:, :], out_offset=None, in_=ysd[:, :], in_offset=bass.IndirectOffsetOnAxis(ap=dsti[:, m, k:k + 1], axis=0),
                                   bounds_check=bc_reg, oob_is_err=False),
                              [t_ysd, dsti], [yk], dma=f"yg{k}{i}")

                for m in range(NC4 - 1):
                    load4c(m)
                for m in range(NOWN):
                    i = m % NC4
                    load4c(m + NC4 - 1)
                    A("dve", V.scalar_tensor_tensor, [y1[i], wall, x2[i]], [x2[i]], out=x2[i][:, :], in0=y1[i][:, :], scalar=wall[:, m, 0:1], in1=x2[i][:, :],
                      op0=ALU.mult, op1=ALU.add)
                    A("pool", POOL.tensor_scalar, [y2[i], wall], [y2[i]], out=y2[i][:, :], in0=y2[i][:, :], scalar1=wall[:, m, 1:2], scalar2=None, op0=ALU.mult)
                    A("dve", V.tensor_tensor, [y2[i], x2[i]], [x2[i]], out=x2[i][:, :], in0=y2[i][:, :], in1=x2[i][:, :], op=ALU.add)
                    DMA(f"x2o{i}", out_d[m * 128:(m + 1) * 128, :], x2[i][:, :], [x2[i]], [t_out], q="act")
                S.barrier()

        nops, nw = S.emit()
        print(f"[build] ops={nops} waits={nw} sems={S.nsem}")
    return nc


def make_in_maps(phases, x, norm_mix, w_in, da_q_norm, da_k_norm, da_lambda_q, da_lambda_k, da_sub_norm,
                 dl_q_norm, dl_k_norm, w_branch_a, w_branch_b, w_out, norm_ffn,
                 w_group_router, w_expert_router, w_gate_up, w_down):
    f = np.float32
    c = make_consts()
    rep = lambda v, n=128: np.ascontiguousarray(np.tile(np.asarray(v, f).reshape(1, -1), (n, 1)))
    pc = lambda v: np.ascontiguousarray(np.asarray(v, f).reshape(16, 128).T)
    shared = dict(c)
    shared.update(
        w_in=np.ascontiguousarray(np.asarray(w_in[0], f)),
        nm=pc(norm_mix[0]), nf=pc(norm_ffn[0]),
        gq_da=rep(da_q_norm[0]), gk_da=rep(da_k_norm[0]), gq_dl=rep(dl_q_norm[0]), gk_dl=rep(dl_k_norm[0]),
        subg=rep(da_sub_norm[0]), lamq=rep(np.asarray(da_lambda_q[0]).reshape(-1)), lamk=rep(np.asarray(da_lambda_k[0]).reshape(-1)),
        w_a=np.ascontiguousarray(np.asarray(w_branch_a[0], f)), w_b=np.ascontiguousarray(np.asarray(w_branch_b[0], f)),
        w_o=np.ascontiguousarray(np.asarray(w_out[0], f)),
        wr=np.ascontiguousarray(np.concatenate([np.asarray(w_group_router[0], f), np.asarray(w_expert_router[0], f)], axis=1)),
    )
    if 4 in phases:
        shared.update(w_gu=np.ascontiguousarray(np.asarray(w_gate_up[0], f)), w_dn=np.ascontiguousarray(np.asarray(w_down[0], f)))
    x = np.asarray(x, f)
    maps = []
    for core in range(8):
        b, j = core // 4, core % 4
        xs = np.zeros((NT * 128, D), f)
        valid = np.zeros((NT, 128), f)
        sh = OWN0 - 16 * j
        xs[sh * 128:] = x[b, :(NT - sh) * 128]
        valid[sh:] = 1.0
        m = dict(shared)
        m["xs"] = xs
        m["valid"] = np.ascontiguousarray(valid.T)
        maps.append(m)
    return maps


def assemble(results):
    out = np.zeros((2, S_LEN, D), np.float32)
    for core in range(8):
        b, j = core // 4, core % 4
        o = np.asarray(results[core]["out"]).reshape(NOWN, 128, D)
        for m in range(NOWN):
            st = 16 * j + m
            out[b, st * 128:(st + 1) * 128] = o[m]
    return out


def kernel(**inputs):
    nc = build()
    maps = make_in_maps((1, 2, 3, 4), **inputs)
    res = run_bass_kernel_spmd(nc, maps, core_ids=list(range(8)))
    return assemble(res.results)
```

```python
import math
from contextlib import ExitStack

import ml_dtypes
import numpy as np

import concourse.bass as bass
import concourse.mybir as mybir
from concourse.bass_utils import run_bass_kernel_spmd

F32 = mybir.dt.float32
F32R = mybir.dt.float32r
BF16 = mybir.dt.bfloat16
I32 = mybir.dt.int32
U32 = mybir.dt.uint32
AF = mybir.ActivationFunctionType
ALU = mybir.AluOpType
AX = mybir.AxisListType

D = 2048
S_LEN = 8192
NT = 64
NOWN = 16
N_IN = 11776
EPS = 1e-6
C_DAQ, C_DAK, C_DAV, C_DLQ, C_DLK, C_DLV, C_GA, C_GB = 0, 1024, 2048, 3072, 4608, 6144, 7680, 9728
NEXP = 32
CAP = 256
DFF = 1024
DL_GROUPS = ((128, 1), (512, 4), (2048, 16))
SAME_ENG_SYNC = True
OWN0 = 48
ALIBI_TH = 44.0
DA_WT = [min(NT, int(math.ceil(ALIBI_TH / (128 * 2.0 ** (-(hh + 1))))) + 1) for hh in range(8)]


class T:
    __slots__ = ("name", "w", "rs")

    def __init__(self, name):
        self.name = name
        self.w = None
        self.rs = []


class Tl:
    def __init__(self, ap, name):
        self.ap = ap
        self.t = T(name)

    def __getitem__(self, k):
        return self.ap[k]


class Op:
    __slots__ = ("eng", "fn", "deps", "sig", "val", "isdma", "sem")


class Sched:
    ENG = ("pe", "act", "dve", "pool", "sp")

    def __init__(self, nc, es):
        self.nc = nc
        self.es = es
        self.e = {"pe": nc.tensor, "act": nc.scalar, "dve": nc.vector, "pool": nc.gpsimd, "sp": nc.sync}
        self.ops = []
        self.last = {k: None for k in self.ENG}
        self.sems = {}
        self.dcount = {}
        self.dma_since = {}
        self.pending = {k: [] for k in self.ENG}
        self.nsem = 0

    def tile(self, es, name, shape, dt):
        ap = es.enter_context(self.nc.sbuf_tensor("sb_" + name, list(shape), dt))
        return Tl(ap, name)

    def psum(self, es, name, shape, dt):
        ap = es.enter_context(self.nc.psum_tensor("ps_" + name, list(shape), dt))
        return Tl(ap, name)

    def dram(self, name):
        return T(name)

    def _sem(self, key):
        if key not in self.sems:
            self.sems[key] = self.es.enter_context(self.nc.semaphore(f"s{self.nsem}"))
            self.nsem += 1
        return self.sems[key]

    def add(self, eng, fn, kw, reads=(), writes=(), dma=None):
        op = Op()
        op.eng = eng
        op.fn = (fn, kw)
        op.sig = False
        op.val = 0
        op.isdma = dma is not None
        op.sem = None
        deps = []
        for r in reads:
            t = r.t if isinstance(r, Tl) else r
            if t.w is not None:
                deps.append(t.w)
        for w in writes:
            t = w.t if isinstance(w, Tl) else w
            if t.w is not None:
                deps.append(t.w)
            deps.extend(t.rs)
        deps.extend(self.pending[eng])
        self.pending[eng] = []
        op.deps = [d for d in dict.fromkeys(deps) if d is not op]
        for r in reads:
            t = r.t if isinstance(r, Tl) else r
            t.rs.append(op)
        for w in writes:
            t = w.t if isinstance(w, Tl) else w
            t.w = op
            t.rs = []
        if op.isdma:
            key = ("dma", dma)
            self._sem(key)
            self.dcount[key] = self.dcount.get(key, 0) + 16
            op.sem = key
            op.val = self.dcount[key]
            self.dma_since[key] = op
        else:
            self.last[eng] = op
        self.ops.append(op)
        return op

    def barrier(self):
        deps = [o for o in self.last.values() if o is not None] + list(self.dma_since.values())
        self.dma_since = {}
        for k in self.ENG:
            self.pending[k] = list(deps)

    def emit(self):
        for op in self.ops:
            for d in op.deps:
                if not d.isdma:
                    if d.eng == op.eng and (d.eng == "pe" or not SAME_ENG_SYNC):
                        continue
                    d.sig = True
        cnt = {k: 0 for k in self.ENG}
        for op in self.ops:
            if not op.isdma and op.sig:
                cnt[op.eng] += 1
                op.val = cnt[op.eng]
                op.sem = ("eng", op.eng)
                self._sem(op.sem)
        waited = {k: {} for k in self.ENG}
        nw = 0
        for op in self.ops:
            E = self.e[op.eng]
            need = {}
            for d in op.deps:
                if not d.isdma and d.eng == op.eng and (d.eng == "pe" or not SAME_ENG_SYNC):
                    continue
                if d.sem is None:
                    continue
                if d.val > need.get(d.sem, 0):
                    need[d.sem] = d.val
            for key, val in need.items():
                if waited[op.eng].get(key, 0) >= val:
                    continue
                waited[op.eng][key] = val
                E.wait_ge(self.sems[key], val)
                nw += 1
            try:
                ins = op.fn[0](**op.fn[1])
            except Exception:
                print("EMIT FAIL", op.eng, getattr(op.fn[0], "__name__", op.fn[0]), {k: (getattr(v, "shape", v), getattr(v, "dtype", None)) for k, v in op.fn[1].items()})
                raise
            if op.isdma:
                ins.then_inc(self.sems[op.sem], 16)
            elif op.sig:
                ins.then_inc(self.sems[op.sem], 1)
        fin = {}
        for op in self.ops:
            if op.sem is not None:
                fin[op.sem] = max(fin.get(op.sem, 0), op.val)
        for key, val in fin.items():
            if waited["sp"].get(key, 0) < val:
                self.nc.sync.wait_ge(self.sems[key], val)
        return len(self.ops), nw


def _bf16(a):
    return np.asarray(a, dtype=np.float32).astype(ml_dtypes.bfloat16)


def make_consts():
    c = {}
    c["ident_f"] = np.eye(128, dtype=np.float32)
    c["ident_b"] = _bf16(np.eye(128))
    kpos = np.arange(S_LEN)
    kb, kr = kpos // 128, kpos % 128
    kaug = np.zeros((8, 4, S_LEN), np.float32)
    for h in range(8):
        sl = 2.0 ** (-(h + 1))
        kaug[h, 0] = sl * 128 * kb
        kaug[h, 1] = sl * kr
        kaug[h, 2] = -sl * 128
        kaug[h, 3] = -sl
    c["kaug"] = _bf16(kaug)
    n = np.arange(NOWN * 128)
    qaug = np.zeros((4, NOWN * 128), np.float32)
    qaug[0] = 1.0
    qaug[1] = 1.0
    qaug[2] = OWN0 + n // 128
    qaug[3] = n % 128
    c["qaug"] = _bf16(qaug)
    dm = np.zeros((128, 4, 512), np.float32)
    ki = np.arange(128)[:, None]
    qi = np.arange(128)[None, :]
    for r in range(4):
        for u in range(4):
            if r > u:
                dm[:, r, u * 128:(u + 1) * 128] = -30000.0
            elif r == u:
                dm[:, r, u * 128:(u + 1) * 128] = np.where(ki <= qi, 0.0, -30000.0)
    c["dmask"] = _bf16(dm)
    tabs = []
    for g, (win, dil) in enumerate(DL_GROUPS):
        no = win // 128 + 1
        for o in range(no):
            dist = 128 * o + qi - ki
            ok = (dist >= 0) & (dist % dil == 0) & (dist <= win)
            tabs.append(np.where(ok, dist, 1e9).astype(np.float32))
    c["distm"] = np.ascontiguousarray(np.stack(tabs, axis=1))
    c["ltri"] = _bf16(np.triu(np.ones((128, 128)), k=1))
    c["ones_b"] = _bf16(np.ones((128, 128)))
    c["eoff"] = np.tile((np.arange(NEXP, dtype=np.float32) * CAP)[None, :], (128, 1))
    return c


DL_SLOPES = [2.0 ** (-8.0 * (h + 1) / 12) for h in range(12)]
DL_NOFF = [w // 128 + 1 for (w, d) in DL_GROUPS]
DL_TBASE = [0, 2, 7]


def build(debug=None, phases=(1, 2, 3, 4)):
    debug = debug or ()
    nc = bass.Bass("TRN2", target_bir_lowering=False)
    nc.dge_precook = False

    def din(name, shape, dt=F32):
        return nc.dram_tensor(name, list(shape), dt, kind="ExternalInput").ap()

    def dscr(name, shape, dt):
        kind = "ExternalOutput" if name in debug else "Internal"
        return nc.dram_tensor(name, list(shape), dt, kind=kind).ap()

    xs = din("xs", [NT * 128, D])
    valid_d = din("valid", [128, NT])
    w_in = din("w_in", [D, N_IN], F32R)
    nm_d = din("nm", [128, 16])
    nf_d = din("nf", [128, 16])
    gq_da_d = din("gq_da", [128, 64])
    gk_da_d = din("gk_da", [128, 64])
    gq_dl_d = din("gq_dl", [128, 128])
    gk_dl_d = din("gk_dl", [128, 128])
    subg_d = din("subg", [128, 128])
    lamq_d = din("lamq", [128, 128])
    lamk_d = din("lamk", [128, 128])
    w_a = din("w_a", [1024, D], F32R)
    w_b = din("w_b", [512, D], F32R)
    w_o = din("w_o", [D, D], F32R)
    wr_d = din("wr", [D, 36])
    if 4 in phases:
        w_gu = din("w_gu", [NEXP, D, 2 * DFF], F32R)
        w_dn = din("w_dn", [NEXP, DFF, D], F32R)
    ident_f_d = din("ident_f", [128, 128])
    ident_b_d = din("ident_b", [128, 128], BF16)
    kaug_d = din("kaug", [8, 4, S_LEN], BF16)
    qaug_d = din("qaug", [4, NOWN * 128], BF16)
    dmask_d = din("dmask", [128, 4, 512], BF16)
    distm_d = din("distm", [128, 24, 128])
    ltri_d = din("ltri", [128, 128], BF16)
    ones_d = din("ones_b", [128, 128], BF16)
    eoff_d = din("eoff", [128, NEXP])

    out_d = nc.dram_tensor("out", [NOWN * 128, D], F32, kind="ExternalOutput").ap()

    kT = dscr("kT", [16, 64, S_LEN], BF16)
    vS = dscr("vS", [8, NT, 128, 130], BF16)
    dkT = dscr("dkT", [12, 128, S_LEN], BF16)
    dvS = dscr("dvS", [12, NT, 128, 130], BF16)
    qT = dscr("qT", [16, 64, NOWN * 128], BF16)
    dqT = dscr("dqT", [12, 128, NOWN * 128], BF16)
    gS = dscr("gS", [NOWN, 128, 4096], F32)
    oA = dscr("oA", [NOWN, 128, 1024], F32)
    oB = dscr("oB", [NOWN, 128, 512], F32)
    mS = dscr("mS", [NOWN, 128, D], F32)
    xsd = dscr("xsd", [NEXP * CAP, D], F32)
    ysd = dscr("ysd", [NEXP * CAP, D], F32)

    es0 = ExitStack()
    with es0:
        S = Sched(nc, es0)
        es0.enter_context(nc.Block())
        E = S.e
        t_kT, t_vS, t_dkT, t_dvS, t_qT, t_dqT = (S.dram(n) for n in ("kT", "vS", "dkT", "dvS", "qT", "dqT"))
        t_gS, t_oA, t_oB, t_mS, t_xsd, t_ysd, t_out = (S.dram(n) for n in ("gS", "oA", "oB", "mS", "xsd", "ysd", "out"))

        def ptile(name, shape, dt=F32):
            return S.tile(es0, name, shape, dt)

        ident_f = ptile("ident_f", [128, 128])
        ident_b = ptile("ident_b", [128, 128], BF16)
        nm = ptile("nm", [128, 16])
        nf = ptile("nf", [128, 16])
        valid = ptile("valid", [128, NT])
        rstd_all = ptile("rstd_all", [128, NT])
        gqk_da = ptile("gqk_da", [128, 64])
        gqk_dl = ptile("gqk_dl", [128, 128])
        subg = ptile("subg", [128, 128])
        neglam = ptile("neglam", [128, 1])
        mhalf = ptile("mhalf", [128, 16])
        junk = ptile("junk", [128, 2048], BF16)
        jt = T("junk_untracked")

        V, ACT, PE, POOL = nc.vector, nc.scalar, nc.tensor, nc.gpsimd

        def A(eng, fn, reads, writes, **kw):
            S.add(eng, fn, kw, reads, writes)

        def DMA(key, out, in_, reads, writes, q="sp"):
            fn = {"sp": nc.sync.dma_start, "act": nc.scalar.dma_start, "pool": nc.gpsimd.dma_start}[q]
            S.add(q, fn, dict(out=out, in_=in_), reads, writes, dma=key)

        def ld(tl, src, key, out=None):
            DMA(key, tl.ap[:] if out is None else out, src, [], [tl])

        ld(ident_f, ident_f_d[:, :], "c0")
        ld(ident_b, ident_b_d[:, :], "c1")
        ld(nm, nm_d[:, :], "c2")
        ld(nf, nf_d[:, :], "c3")
        ld(valid, valid_d[:, :], "c4")
        ld(subg, subg_d[:, :], "c5")
        A("pool", POOL.memset, [], [mhalf], ap=mhalf.ap[:], constant=-0.5)
        with ExitStack() as es:
            ta = S.tile(es, "tmpa", [128, 128], F32)
            tb = S.tile(es, "tmpb", [128, 128], F32)
            e2 = S.tile(es, "tmpe", [128, 2], F32)
            ld(ta, gq_da_d[:, :], "c6", out=ta[:, 0:64])
            ld(tb, gk_da_d[:, :], "c7", out=tb[:, 0:64])
            A("dve", V.scalar_tensor_tensor, [ta, tb], [gqk_da], out=gqk_da.ap[:], in0=ta[:, 0:64], scalar=0.125, in1=tb[:, 0:64],
              op0=ALU.mult, op1=ALU.mult)
            ld(ta, gq_dl_d[:, :], "c6")
            ld(tb, gk_dl_d[:, :], "c7")
            A("dve", V.scalar_tensor_tensor, [ta, tb], [gqk_dl], out=gqk_dl.ap[:], in0=ta[:, :], scalar=128 ** -0.5, in1=tb[:, :],
              op0=ALU.mult, op1=ALU.mult)
            ld(ta, lamq_d[:, :], "c6")
            ld(tb, lamk_d[:, :], "c7")
            for i in range(2):
                A("dve", V.scalar_tensor_tensor, [ta, tb], [e2], out=junk[:, 0:64], in0=ta[:, i * 64:(i + 1) * 64], scalar=1.0,
                  in1=tb[:, i * 64:(i + 1) * 64], op0=ALU.mult, op1=ALU.mult, accum_out=e2[:, i:i + 1])
            A("act", ACT.activation, [e2], [e2], out=e2[:, :], in_=e2[:, :], func=AF.Exp)
            A("dve", V.scalar_tensor_tensor, [e2], [neglam], out=neglam.ap[:], in0=e2[:, 1:2], scalar=-0.2, in1=e2[:, 0:1],
              op0=ALU.add, op1=ALU.subtract)
            A("dve", V.tensor_scalar, [subg], [subg], out=subg.ap[:], in0=subg.ap[:], scalar1=0.8, scalar2=None, op0=ALU.mult)
            S.barrier()

        def rsqrt_ops(ss, rs_ap, n, inv, rs_tl):
            A("dve", V.tensor_scalar, [ss], [ss], out=ss[:, 0:n], in0=ss[:, 0:n], scalar1=inv, scalar2=EPS, op0=ALU.mult, op1=ALU.add)
            A("pool", POOL.tensor_tensor, [ss, mhalf], [rs_tl], out=rs_ap, in0=ss[:, 0:n], in1=mhalf[:, 0:n], op=ALU.pow)

        def sumsq(src_tl, src_ap, n, acc_tl, acc_ap):
            A("dve", V.scalar_tensor_tensor, [src_tl], [acc_tl], out=junk[:, 0:n], in0=src_ap, scalar=1.0, in1=src_ap,
              op0=ALU.mult, op1=ALU.mult, accum_out=acc_ap)

        if 1 in phases:
            with ExitStack() as es:
                TB = 8
                NB3 = 3
                xt = [S.tile(es, f"xt{i}", [128, D], F32) for i in range(2)]
                xT = [S.tile(es, f"xT{i}", [128, 16, 128], F32R) for i in range(TB)]
                wb = [S.tile(es, f"wb{i}", [128, 16, 512], F32R) for i in range(2)]
                pr = [S.tile(es, f"pr{i}", [128, 512], F32) for i in range(NB3)]
                sq = [S.tile(es, f"sq{i}", [128, 512], F32) for i in range(NB3)]
                kn = [S.tile(es, f"kn{i}", [128, 512], BF16) for i in range(NB3)]
                kst = [S.tile(es, f"kst{i}", [128, 8, 128], BF16) for i in range(2)]
                vst = [S.tile(es, f"vst{i}", [128, 4, 130], BF16) for i in range(2)]
                ssq = [S.tile(es, f"ssq{i}", [128, 8], F32) for i in range(NB3)]
                rsq = [S.tile(es, f"rsq{i}", [128, 8], F32) for i in range(NB3)]
                xss = [S.tile(es, f"xss{i}", [128, 1], F32) for i in range(2)]
                ps_mm = [S.psum(es, f"pmm{i}", [128, 512], F32) for i in range(2)]
                ps_tx = [S.psum(es, f"ptx{i}", [128, 4, 128], F32) for i in range(2)]
                ps_tk = [S.psum(es, f"ptk{i}", [128, 8, 128], BF16) for i in range(2)]
                rstd_t = [T(f"rstd{p}") for p in range(NT)]
                for v in vst:
                    A("pool", POOL.memset, [], [v], ap=v.ap[:], constant=0.0)
                ctr = {"x": 0, "w": 0, "mm": 0, "tx": 0, "pp": 0, "tk": 0, "v": 0}

                xseq = [(p, True) for p in range(NT)]
                xbuf = {}

                def issue_x(i):
                    if i >= len(xseq):
                        return
                    xi = xt[i % 2]
                    xbuf[i] = xi
                    p = xseq[i][0]
                    ld(xi, xs[p * 128:(p + 1) * 128, :], "x" + xi.t.name)

                def do_xT(i, slot):
                    p, need_rstd = xseq[i]
                    xi = xbuf[i]
                    if need_rstd:
                        sx = xss[i % 2]
                        sumsq(xi, xi[:, :], D, sx, sx[:, 0:1])
                        rsqrt_ops(sx, rstd_all[:, p:p + 1], 1, 1.0 / D, rstd_t[p])
                    for q in range(4):
                        bank = ps_tx[ctr["tx"] % 2]
                        ctr["tx"] += 1
                        for k in range(4):
                            c = 4 * q + k
                            A("pe", PE.transpose, [xi, ident_f], [bank], out=bank[:, k, :], in_=xi[:, c * 128:(c + 1) * 128], identity=ident_f[:, :])
                        A("dve", V.tensor_tensor, [bank, nm], [xT[slot]], out=xT[slot][:, 4 * q:4 * q + 4, :], in0=bank[:, :, :],
                          in1=nm[:, 4 * q:4 * q + 4].unsqueeze(2).to_broadcast([128, 4, 128]), op=ALU.mult)
                    issue_x(i + 2)

                def mm(slot, w):
                    acc = ps_mm[ctr["mm"] % 2]
                    ctr["mm"] += 1
                    for c in range(16):
                        A("pe", PE.matmul, [xT[slot], w], [acc], out=acc[:, :], lhsT=xT[slot][:, c, :], rhs=w[:, c, :],
                          start=(c == 0), stop=(c == 15))
                    return acc

                def post_norm(acc, p, G, W, gain, dst, dst_t, tok0):
                    i = ctr["pp"] % NB3
                    ctr["pp"] += 1
                    prt, sqt, knt, sst, rst = pr[i], sq[i], kn[i], ssq[i], rsq[i]
                    A("act", ACT.activation, [acc, rstd_t[p]], [prt], out=prt[:, :], in_=acc[:, :], func=AF.Copy, scale=rstd_all[:, p:p + 1])
                    A("pool", POOL.tensor_tensor, [prt], [sqt], out=sqt[:, :], in0=prt[:, :], in1=prt[:, :], op=ALU.mult)
                    A("dve", V.tensor_reduce, [sqt], [sst], out=sst[:, 0:G], in_=sqt[:, :].rearrange("p (g w) -> p g w", g=G), axis=AX.X, op=ALU.add)
                    rsqrt_ops(sst, rst[:, 0:G], G, 1.0 / W, rst)
                    pv = prt[:, :].rearrange("p (g w) -> p g w", g=G)
                    kv = knt[:, :].rearrange("p (g w) -> p g w", g=G)
                    rb = rst[:, 0:G].unsqueeze(2).to_broadcast([128, G, W])
                    if gain is None:
                        A("dve", V.tensor_tensor, [prt, rst], [knt], out=kv, in0=pv, in1=rb, op=ALU.mult)
                    else:
                        A("dve", V.tensor_tensor, [prt, rst], [prt], out=pv, in0=pv, in1=rb, op=ALU.mult)
                        A("pool", POOL.tensor_tensor, [prt, gain], [knt], out=kv, in0=pv,
                          in1=gain[:, 0:W].unsqueeze(1).to_broadcast([128, G, W]), op=ALU.mult)

                    def part_b():
                        j = ctr["tk"] % 2
                        ctr["tk"] += 1
                        kstt, ptk = kst[j], ps_tk[j]
                        for g in range(G):
                            A("pe", PE.transpose, [knt, ident_b], [ptk], out=ptk[0:W, g, :], in_=knt[:, g * W:(g + 1) * W], identity=ident_b[:, :])
                        A("act", ACT.copy, [ptk], [kstt], out=kstt[0:W, 0:G, :], in_=ptk[0:W, 0:G, :])
                        DMA("st" + kstt.t.name, dst[:, :, tok0:tok0 + 128].rearrange("h d t -> d h t"), kstt[0:W, 0:G, :], [kstt], [dst_t], q="act")
                    return part_b

                def post_v(acc, p, dst, dst_t):
                    v = vst[ctr["v"] % 2]
                    ctr["v"] += 1
                    A("act", ACT.activation, [acc, rstd_t[p]], [v], out=v[:, :, 0:128], in_=acc[:, :].rearrange("p (h c) -> p h c", h=4),
                      func=AF.Copy, scale=rstd_all[:, p:p + 1])
                    A("pool", POOL.tensor_copy, [valid], [v], out=v[:, :, 128:129], in_=valid[:, p:p + 1].unsqueeze(1).to_broadcast([128, 4, 1]))
                    DMA("st" + v.t.name, dst[:, p, :, :].rearrange("h p c -> p h c"), v[:, :, :], [v], [dst_t], q="act")
                    return None

                def post_gate(acc, p, m, gb):
                    i = ctr["pp"] % NB3
                    ctr["pp"] += 1
                    prt = pr[i]
                    A("act", ACT.activation, [acc, rstd_t[p]], [prt], out=prt[:, :], in_=acc[:, :], func=AF.Copy, scale=rstd_all[:, p:p + 1])
                    DMA("st" + prt.t.name, gS[m, :, gb * 512:(gb + 1) * 512], prt[:, :], [prt], [t_gS], q="act")
                    return None

                kv_blocks = ([("dak", b_) for b_ in range(2)] + [("dav", b_) for b_ in range(2)] + [("dlk", g) for g in range(3)]
                             + [("dlv", g) for g in range(3)])
                q_blocks = [("daq", b_) for b_ in range(2)] + [("dlq", g) for g in range(3)] + [("gate", g) for g in range(8)]
                col_of = {"dak": C_DAK, "dav": C_DAV, "dlk": C_DLK, "dlv": C_DLV, "daq": C_DAQ, "dlq": C_DLQ, "gate": C_GA}

                def first_tile(kind, bi):
                    if kind in ("dak", "dav"):
                        return max(0, OWN0 - max(DA_WT[4 * bi:4 * bi + 4]))
                    if kind in ("dlk", "dlv"):
                        return OWN0 - (DL_NOFF[bi] - 1)
                    return OWN0
                sbs = []
                for sb in range(NT // TB):
                    tiles = [sb * TB + tl for tl in range(TB)]
                    blocks = [(k_, b_) for (k_, b_) in kv_blocks + q_blocks if first_tile(k_, b_) <= tiles[-1]]
                    sbs.append((tiles, blocks))
                wseq = [(si, kind, bi) for si, (tiles, blocks) in enumerate(sbs) for (kind, bi) in blocks]
                wtile = {}

                def issue_w(i):
                    if i >= len(wseq):
                        return
                    w = wb[i % 2]
                    wtile[i] = w
                    col0 = col_of[wseq[i][1]] + 512 * wseq[i][2]
                    ld(w, w_in[:, col0:col0 + 512].rearrange("(c p) n -> p c n", p=128), "w" + w.t.name)

                pend = []
                issue_x(0)
                issue_x(1)
                issue_w(0)
                for tl in range(TB):
                    do_xT(tl, tl)
                wi = 0
                for si, (tiles, blocks) in enumerate(sbs):
                    for bk, (kind, bi) in enumerate(blocks):
                        w = wtile[wi]
                        issue_w(wi + 1)
                        wi += 1
                        ft = first_tile(kind, bi)
                        for tl, p in enumerate(tiles):
                            m = p - OWN0
                            if p >= ft:
                                acc = mm(tl, w)
                                if len(pend) >= 2:
                                    pb = pend.pop(0)
                                    if pb is not None:
                                        pb()
                                if kind == "dak":
                                    pb = post_norm(acc, p, 8, 64, None, kT[8 * bi:8 * bi + 8], t_kT, p * 128)
                                elif kind == "dlk":
                                    pb = post_norm(acc, p, 4, 128, None, dkT[4 * bi:4 * bi + 4], t_dkT, p * 128)
                                elif kind == "dav":
                                    pb = post_v(acc, p, vS[4 * bi:4 * bi + 4], t_vS)
                                elif kind == "dlv":
                                    pb = post_v(acc, p, dvS[4 * bi:4 * bi + 4], t_dvS)
                                elif kind == "daq":
                                    pb = post_norm(acc, p, 8, 64, gqk_da, qT[8 * bi:8 * bi + 8], t_qT, m * 128)
                                elif kind == "dlq":
                                    pb = post_norm(acc, p, 4, 128, gqk_dl, dqT[4 * bi:4 * bi + 4], t_dqT, m * 128)
                                else:
                                    pb = post_gate(acc, p, m, bi)
                                pend.append(pb)
                            if bk == len(blocks) - 1 and si + 1 < len(sbs):
                                do_xT((si + 1) * TB + tl, tl)
                for pb in pend:
                    if pb is not None:
                        pb()
                S.barrier()

        if 2 in phases:
            with ExitStack() as es:
                kth = [S.tile(es, f"kth{i}", [68, 2, S_LEN], BF16) for i in range(2)]
                qth = [S.tile(es, f"qth{i}", [68, 2, NOWN * 128], BF16) for i in range(2)]
                vh = [S.tile(es, f"vh{i}", [128, NT, 130], BF16) for i in range(2)]
                mk = S.tile(es, "mk", [128, 4, 512], BF16)
                pt = [S.tile(es, f"pt{k}", [128, 2, 512], BF16) for k in range(2)]
                oah = [S.tile(es, f"oah{i}", [128, NOWN, 128], F32) for i in range(2)]
                fr = [S.tile(es, f"fr{i}", [128, 4], F32) for i in range(2)]
                ft = [S.tile(es, f"ft{i}", [128, 128], F32) for i in range(2)]
                fo = [S.tile(es, f"fo{i}", [128, 128], F32) for i in range(2)]
                ps_s = [S.psum(es, f"pss{k}", [128, 2, 512], F32) for k in range(2)]
                ps_o = [S.psum(es, f"pso{k}", [128, 3, 130], F32) for k in range(3)]
                ld(mk, dmask_d[:, :, :], "mk")

                def load_head(h):
                    hb = h % 2
                    c0 = max(0, OWN0 - DA_WT[h]) * 128
                    t0 = c0 // 128
                    for i in range(2):
                        DMA(f"kth{hb}", kth[hb][0:64, i, c0:], kT[2 * h + i, :, c0:], [t_kT], [kth[hb]])
                        DMA(f"kth{hb}", kth[hb][64:68, i, c0:], kaug_d[h, :, c0:], [], [kth[hb]])
                        DMA(f"qth{hb}", qth[hb][0:64, i, :], qT[2 * h + i, :, :], [t_qT], [qth[hb]])
                        DMA(f"qth{hb}", qth[hb][64:68, i, :], qaug_d[:, :], [], [qth[hb]])
                    DMA(f"vh{hb}", vh[hb][:, t0:, :], vS[h, t0:].rearrange("t p c -> p t c"), [t_vS], [vh[hb]])

                load_head(0)
                fin = 0
                for h in range(8):
                    hb = h % 2
                    if h + 1 < 8:
                        load_head(h + 1)
                    for a in range(4):
                        d0 = OWN0 + 4 * a
                        lo = max(0, d0 - DA_WT[h])
                        kts = list(range(lo, d0 + 4))

                        def qk(kt):
                            for i in range(2):
                                s_ = ps_s[kt % 2]
                                diag = kt >= d0
                                A("pe", PE.matmul, [kth[hb], qth[hb]], [s_], out=s_[:, i, :], lhsT=kth[hb][0:68, i, kt * 128:(kt + 1) * 128],
                                  rhs=qth[hb][0:68, i, a * 512:(a + 1) * 512], start=True, stop=not diag)
                                if diag:
                                    A("pe", PE.matmul, [ident_b, mk], [s_], out=s_[:, i, :], lhsT=ident_b[:, :], rhs=mk[:, kt - d0, :],
                                      start=False, stop=True)

                        qk(kts[0])
                        for kt in kts:
                            if kt + 1 <= kts[-1]:
                                qk(kt + 1)
                            A("act", ACT.activation, [ps_s[kt % 2]], [pt[kt % 2]], out=pt[kt % 2][:, :, :], in_=ps_s[kt % 2][:, :, :], func=AF.Exp)
                            for i in range(2):
                                for u in range(4):
                                    last = d0 + u
                                    if kt > last:
                                        continue
                                    idx = i * 4 + u
                                    bank = ps_o[idx // 3]
                                    A("pe", PE.matmul, [pt[kt % 2], vh[hb]], [bank], out=bank[:, idx % 3, :],
                                      lhsT=pt[kt % 2][:, i, u * 128:(u + 1) * 128], rhs=vh[hb][:, kt, :],
                                      start=(kt == lo and idx % 3 == 0), stop=(kt == last), skip_group_check=True)
                        for u in range(4):
                            m = 4 * a + u
                            f = fin % 2
                            fin += 1
                            b0, b1 = ps_o[u // 3], ps_o[(4 + u) // 3]
                            a0, a1 = b0[:, u % 3, :], b1[:, (4 + u) % 3, :]
                            A("dve", V.reciprocal, [b0], [fr[f]], out=fr[f][:, 0:1], in_=a0[:, 128:129])
                            A("dve", V.reciprocal, [b1], [fr[f]], out=fr[f][:, 1:2], in_=a1[:, 128:129])
                            A("dve", V.tensor_scalar, [b1, fr[f], neglam], [ft[f]], out=ft[f][:, :], in0=a1[:, 0:128], scalar1=fr[f][:, 1:2],
                              scalar2=neglam[:, 0:1], op0=ALU.mult, op1=ALU.mult)
                            A("dve", V.scalar_tensor_tensor, [b0, fr[f], ft[f]], [fo[f]], out=fo[f][:, :], in0=a0[:, 0:128], scalar=fr[f][:, 0:1],
                              in1=ft[f][:, :], op0=ALU.mult, op1=ALU.add)
                            sumsq(fo[f], fo[f][:, :], 128, fr[f], fr[f][:, 2:3])
                            A("dve", V.tensor_scalar, [fr[f]], [fr[f]], out=fr[f][:, 2:3], in0=fr[f][:, 2:3], scalar1=1.0 / 128, scalar2=EPS,
                              op0=ALU.mult, op1=ALU.add)
                            A("pool", POOL.tensor_tensor, [fr[f], mhalf], [fr[f]], out=fr[f][:, 3:4], in0=fr[f][:, 2:3], in1=mhalf[:, 0:1], op=ALU.pow)
                            A("dve", V.scalar_tensor_tensor, [fo[f], fr[f], subg], [oah[hb]], out=oah[hb][:, m, :], in0=fo[f][:, :],
                              scalar=fr[f][:, 3:4], in1=subg[:, :], op0=ALU.mult, op1=ALU.mult)
                    DMA(f"oah{hb}", oA[:, :, h * 128:(h + 1) * 128].rearrange("m p c -> p m c"), oah[hb][:, :, :], [oah[hb]], [t_oA])
                S.barrier()

            with ExitStack() as es:
                NR = 4
                kd = [S.tile(es, f"kd{g}", [128, S_LEN], BF16) for g in range(3)]
                vd = [S.tile(es, f"vd{g}", [128, NT, 130], BF16) for g in range(3)]
                qd = [S.tile(es, f"qd{g}", [128, NOWN * 128], BF16) for g in range(3)]
                dist = S.tile(es, "dist", [128, 24, 128], F32)
                sc = [S.tile(es, f"sc{i}", [128, 512], F32) for i in range(NR)]
                ptd = [S.tile(es, f"ptd{i}", [128, 512], BF16) for i in range(NR)]
                obh = [S.tile(es, f"obh{i}", [128, NOWN, 128], F32) for i in range(2)]
                frd = [S.tile(es, f"frd{i}", [128, 1], F32) for i in range(4)]
                ps_sd = [S.psum(es, f"psd{i}", [128, 512], F32) for i in range(NR)]
                ps_od = [S.psum(es, f"pod{i}", [128, 130], F32) for i in range(4)]
                ld(dist, distm_d[:, :, :], "dist")
                for h in range(4):
                    hb = h % 2
                    for g in range(3):
                        gh = g * 4 + h
                        DMA(f"kd{g}", kd[g][:, 32 * 128:], dkT[gh, :, 32 * 128:], [t_dkT], [kd[g]])
                        DMA(f"vd{g}", vd[g][:, 32:, :], dvS[gh, 32:].rearrange("t p c -> p t c"), [t_dvS], [vd[g]])
                        DMA(f"qd{g}", qd[g][:, :], dqT[gh, :, :], [t_dqT], [qd[g]])
                    for qg in range(4):
                        p0 = OWN0 + 4 * qg
                        steps = []
                        for g in range(3):
                            no = DL_NOFF[g]
                            for kp in range(p0 - (no - 1), p0 + 4):
                                ulo, uhi = max(0, kp - p0), min(3, kp - p0 + no - 1)
                                steps.append((g, kp, ulo, uhi))
                        first_u, last_u = {}, {}
                        for si_, (g, kp, ulo, uhi) in enumerate(steps):
                            for u in range(ulo, uhi + 1):
                                first_u.setdefault(u, si_)
                                last_u[u] = si_

                        def dqk(si_):
                            g, kp, ulo, uhi = steps[si_]
                            s_ = ps_sd[si_ % NR]
                            A("pe", PE.matmul, [kd[g], qd[g]], [s_], out=s_[:, ulo * 128:(uhi + 1) * 128], lhsT=kd[g][:, kp * 128:(kp + 1) * 128],
                              rhs=qd[g][:, (4 * qg + ulo) * 128:(4 * qg + uhi + 1) * 128], start=True, stop=True)

                        dqk(0)
                        dqk(1)
                        dqk(2)
                        for si_, (g, kp, ulo, uhi) in enumerate(steps):
                            if si_ + 3 < len(steps):
                                dqk(si_ + 3)
                            r_ = si_ % NR
                            nu = uhi - ulo + 1
                            o0 = p0 + ulo - kp
                            A("dve", V.scalar_tensor_tensor, [dist, ps_sd[r_]], [sc[r_]], out=sc[r_][:, ulo * 128:(uhi + 1) * 128].rearrange("p (u c) -> p u c", u=nu),
                              in0=dist[:, DL_TBASE[g] + o0:DL_TBASE[g] + o0 + nu, :], scalar=-DL_SLOPES[g * 4 + h],
                              in1=ps_sd[r_][:, ulo * 128:(uhi + 1) * 128].rearrange("p (u c) -> p u c", u=nu), op0=ALU.mult, op1=ALU.add)
                            A("act", ACT.activation, [sc[r_]], [ptd[r_]], out=ptd[r_][:, ulo * 128:(uhi + 1) * 128], in_=sc[r_][:, ulo * 128:(uhi + 1) * 128],
                              func=AF.Exp)
                            for u in range(ulo, uhi + 1):
                                A("pe", PE.matmul, [ptd[r_], vd[g]], [ps_od[u]], out=ps_od[u][:, :], lhsT=ptd[r_][:, u * 128:(u + 1) * 128], rhs=vd[g][:, kp, :],
                                  start=(first_u[u] == si_), stop=(last_u[u] == si_))
                        for u in range(4):
                            m = 4 * qg + u
                            A("dve", V.reciprocal, [ps_od[u]], [frd[u]], out=frd[u][:, 0:1], in_=ps_od[u][:, 128:129])
                            A("dve", V.tensor_scalar, [ps_od[u], frd[u]], [obh[hb]], out=obh[hb][:, m, :], in0=ps_od[u][:, 0:128], scalar1=frd[u][:, 0:1],
                              scalar2=None, op0=ALU.mult)
                    DMA(f"obh{hb}", oB[:, :, h * 128:(h + 1) * 128].rearrange("m p c -> p m c"), obh[hb][:, :, :], [obh[hb]], [t_oB])
                S.barrier()

        if 3 in phases:
            with ExitStack() as es:
                wa = S.tile(es, "wa", [128, 8, D], F32R)
                wbb = S.tile(es, "wbb", [128, 4, D], F32R)
                ot = [S.tile(es, f"ot{i}", [128, 1536], F32) for i in range(2)]
                oT = [S.tile(es, f"oT{i}", [128, 12, 128], F32R) for i in range(2)]
                gt = [S.tile(es, f"gt{i}", [128, 4096], F32) for i in range(2)]
                mx = [S.tile(es, f"mx{i}", [128, D], F32) for i in range(2)]
                t1 = [S.tile(es, f"t1{i}", [128, 512], F32) for i in range(2)]
                t2 = [S.tile(es, f"t2{i}", [128, 512], F32) for i in range(2)]
                ps_t = [S.psum(es, f"p3t{i}", [128, 4, 128], F32) for i in range(2)]
                ps_a = [S.psum(es, f"p3a{i}", [128, 512], F32) for i in range(2)]
                ps_b = [S.psum(es, f"p3b{i}", [128, 512], F32) for i in range(2)]

                def load3(m):
                    i = m % 2
                    DMA(f"ot{i}", ot[i][:, 0:1024], oA[m, :, :], [t_oA], [ot[i]])
                    DMA(f"ot{i}", ot[i][:, 1024:1536], oB[m, :, :], [t_oB], [ot[i]])
                    DMA(f"gt{i}", gt[i][:, :], gS[m, :, :], [t_gS], [gt[i]])

                def prep3(m):
                    i = m % 2
                    for q in range(3):
                        bank = ps_t[q % 2]
                        for k in range(4):
                            c = 4 * q + k
                            A("pe", PE.transpose, [ot[i], ident_f], [bank], out=bank[:, k, :], in_=ot[i][:, c * 128:(c + 1) * 128], identity=ident_f[:, :])
                        A("act", ACT.copy, [bank], [oT[i]], out=oT[i][:, 4 * q:4 * q + 4, :], in_=bank[:, :, :])
                    A("act", ACT.activation, [gt[i]], [gt[i]], out=gt[i][:, :], in_=gt[i][:, :], func=AF.Sigmoid)

                load3(0)
                load3(1)
                DMA("wa", wa[:, :, :], w_a.rearrange("(c p) n -> p c n", p=128), [], [wa])
                DMA("wbb", wbb[:, :, :], w_b.rearrange("(c p) n -> p c n", p=128), [], [wbb])
                prep3(0)
                k3 = 0
                for m in range(NOWN):
                    i = m % 2
                    if m + 1 < NOWN:
                        prep3(m + 1)
                    for cb in range(4):
                        pa, pb = ps_a[k3 % 2], ps_b[k3 % 2]
                        ta_, tb_ = t1[k3 % 2], t2[k3 % 2]
                        k3 += 1
                        for c in range(8):
                            A("pe", PE.matmul, [oT[i], wa], [pa], out=pa[:, :], lhsT=oT[i][:, c, :], rhs=wa[:, c, cb * 512:(cb + 1) * 512],
                              start=(c == 0), stop=(c == 7))
                        for c in range(4):
                            A("pe", PE.matmul, [oT[i], wbb], [pb], out=pb[:, :], lhsT=oT[i][:, 8 + c, :], rhs=wbb[:, c, cb * 512:(cb + 1) * 512],
                              start=(c == 0), stop=(c == 3))
                        A("dve", V.tensor_tensor, [pa, gt[i]], [ta_], out=ta_[:, :], in0=pa[:, :], in1=gt[i][:, cb * 512:(cb + 1) * 512], op=ALU.mult)
                        A("dve", V.tensor_tensor, [pb, gt[i]], [tb_], out=tb_[:, :], in0=pb[:, :], in1=gt[i][:, 2048 + cb * 512:2048 + (cb + 1) * 512],
                          op=ALU.mult)
                        A("pool", POOL.tensor_tensor, [ta_, tb_], [mx[i]], out=mx[i][:, cb * 512:(cb + 1) * 512], in0=ta_[:, :], in1=tb_[:, :], op=ALU.add)
                    DMA(f"mx{i}", mS[m, :, :], mx[i][:, :], [mx[i]], [t_mS], q="act")
                    if m + 2 < NOWN:
                        load3(m + 2)
                S.barrier()

            with ExitStack() as es:
                TB = 8
                mt = [S.tile(es, f"mt{i}", [128, D], F32) for i in range(2)]
                mT = [S.tile(es, f"mT{i}", [128, 16, 128], F32R) for i in range(TB)]
                wo = [S.tile(es, f"wo{i}", [128, 16, 512], F32R) for i in range(2)]
                xo = [S.tile(es, f"xo{i}", [128, 512], F32) for i in range(3)]
                rs_ = [S.tile(es, f"rs{i}", [128, 512], F32) for i in range(2)]
                ps_t = [S.psum(es, f"p4t{i}", [128, 4, 128], F32) for i in range(2)]
                ps_a = [S.psum(es, f"p4a{i}", [128, 512], F32) for i in range(2)]
                k3 = 0
                wlist = [(sb, cb) for sb in range(NOWN // TB) for cb in range(4)]
                wot = {}

                def issue_wo(i):
                    if i >= len(wlist):
                        return
                    w = wo[i % 2]
                    wot[i] = w
                    cb = wlist[i][1]
                    DMA("wo" + w.t.name, w[:, :, :], w_o[:, cb * 512:(cb + 1) * 512].rearrange("(c p) n -> p c n", p=128), [], [w])

                def load_mt(m):
                    if m < NOWN:
                        DMA(f"mt{m % 2}", mt[m % 2][:, :], mS[m, :, :], [t_mS], [mt[m % 2]])

                issue_wo(0)
                load_mt(0)
                load_mt(1)
                wi = 0
                for sb in range(NOWN // TB):
                    for tl in range(TB):
                        m = sb * TB + tl
                        i = m % 2
                        for q in range(4):
                            bank = ps_t[q % 2]
                            for k in range(4):
                                c = 4 * q + k
                                A("pe", PE.transpose, [mt[i], ident_f], [bank], out=bank[:, k, :], in_=mt[i][:, c * 128:(c + 1) * 128], identity=ident_f[:, :])
                            A("act", ACT.copy, [bank], [mT[tl]], out=mT[tl][:, 4 * q:4 * q + 4, :], in_=bank[:, :, :])
                        load_mt(m + 2)
                    for cb in range(4):
                        w = wot[wi]
                        issue_wo(wi + 1)
                        wi += 1
                        for tl in range(TB):
                            m = sb * TB + tl
                            p = OWN0 + m
                            acc, xo_, r_ = ps_a[k3 % 2], xo[k3 % 3], rs_[k3 % 2]
                            k3 += 1
                            DMA("xo" + xo_.t.name, xo_[:, :], xs[p * 128:(p + 1) * 128, cb * 512:(cb + 1) * 512], [], [xo_])
                            for c in range(16):
                                A("pe", PE.matmul, [mT[tl], w], [acc], out=acc[:, :], lhsT=mT[tl][:, c, :], rhs=w[:, c, :], start=(c == 0), stop=(c == 15))
                            A("dve", V.tensor_tensor, [acc, xo_], [r_], out=r_[:, :], in0=acc[:, :], in1=xo_[:, :], op=ALU.add)
                            DMA("rs" + r_.t.name, out_d[m * 128:(m + 1) * 128, cb * 512:(cb + 1) * 512], r_[:, :], [r_], [t_out], q="act")
                S.barrier()

        if 4 in phases:
            dsti = ptile("dsti", [128, NOWN, 2], I32)
            wall = ptile("wall", [128, NOWN, 2], F32)
            bc_reg = nc.gpsimd.alloc_register("bc")
            S.add("pool", POOL.reg_mov, dict(out_reg=bc_reg, val=NEXP * CAP - 1), [], [])
            with ExitStack() as es:
                NIL = 4
                x2 = [S.tile(es, f"x2{i}", [128, D], F32) for i in range(NIL)]
                xn = [S.tile(es, f"xn{i}", [128, D], F32) for i in range(NIL)]
                xnT = [S.tile(es, f"xnT{i}", [128, 16, 128], F32) for i in range(NIL)]
                wr = S.tile(es, "wr", [128, 16, 36], F32)
                ltri = S.tile(es, "ltri", [128, 128], BF16)
                onesb = S.tile(es, "onesb", [128, 128], BF16)
                eoff = S.tile(es, "eoff", [128, NEXP], F32)
                tot = S.tile(es, "tot", [128, NEXP], F32)
                sm = [S.tile(es, f"sm{i}", [128, 16], F32) for i in range(NIL)]
                lg = [S.tile(es, f"lg{i}", [128, 36], F32) for i in range(NIL)]
                ge = [S.tile(es, f"ge{i}", [128, 4], F32) for i in range(NIL)]
                gm = [S.tile(es, f"gm{i}", [128, 4], F32) for i in range(NIL)]
                elm = [S.tile(es, f"elm{i}", [128, NEXP], F32) for i in range(NIL)]
                mx8 = [S.tile(es, f"mx8{i}", [128, 8], F32) for i in range(NIL)]
                M1 = [S.tile(es, f"M1{i}", [128, NEXP], F32) for i in range(NIL)]
                M2 = [S.tile(es, f"M2{i}", [128, NEXP], F32) for i in range(NIL)]
                Mb = [S.tile(es, f"Mb{i}", [128, NEXP], BF16) for i in range(NIL)]
                slot = [S.tile(es, f"slot{i}", [128, NEXP], F32) for i in range(NIL)]
                ovf = [S.tile(es, f"ovf{i}", [128, NEXP], F32) for i in range(NIL)]
                ps_t = [S.psum(es, f"p5t{i}", [128, 4, 128], F32) for i in range(4)]
                ps_l = [S.psum(es, f"p5l{i}", [128, 36], F32) for i in range(2)]
                ps_r = [S.psum(es, f"p5r{i}", [128, 2, NEXP], F32) for i in range(2)]
                ld(wr, wr_d.rearrange("(c p) n -> p c n", p=128), "wr")
                ld(ltri, ltri_d[:, :], "ltri")
                ld(onesb, ones_d[:, :], "onesb")
                ld(eoff, eoff_d[:, :], "eoff")
                A("dve", V.memset, [], [tot], ap=tot.ap[:], constant=0.0)

                def route_tile(m):
                    i = m % NIL
                    s_ = sm[i]
                    pl, prk = ps_l[m % 2], ps_r[m % 2]
                    DMA(f"x2{i}", x2[i][:, :], out_d[m * 128:(m + 1) * 128, :], [t_out], [x2[i]])
                    sumsq(x2[i], x2[i][:, :], D, s_, s_[:, 0:1])
                    yield
                    A("dve", V.tensor_scalar, [s_], [s_], out=s_[:, 0:1], in0=s_[:, 0:1], scalar1=1.0 / D, scalar2=EPS, op0=ALU.mult, op1=ALU.add)
                    A("pool", POOL.tensor_tensor, [s_, mhalf], [s_], out=s_[:, 1:2], in0=s_[:, 0:1], in1=mhalf[:, 0:1], op=ALU.pow)
                    A("act", ACT.activation, [x2[i], s_], [xn[i]], out=xn[i][:, :], in_=x2[i][:, :], func=AF.Copy, scale=s_[:, 1:2])
                    yield
                    for q in range(4):
                        bank = ps_t[q]
                        for k in range(4):
                            c = 4 * q + k
                            A("pe", PE.transpose, [xn[i], ident_f], [bank], out=bank[:, k, :], in_=xn[i][:, c * 128:(c + 1) * 128], identity=ident_f[:, :])
                        A("dve", V.tensor_tensor, [bank, nf], [xnT[i]], out=xnT[i][:, 4 * q:4 * q + 4, :], in0=bank[:, :, :],
                          in1=nf[:, 4 * q:4 * q + 4].unsqueeze(2).to_broadcast([128, 4, 128]), op=ALU.mult)
                        yield
                    for c in range(16):
                        A("pe", PE.matmul, [xnT[i], wr], [pl], out=pl[:, :], lhsT=xnT[i][:, c, :], rhs=wr[:, c, :], start=(c == 0), stop=(c == 15))
                    A("dve", V.tensor_copy, [pl], [lg[i]], out=lg[i][:, :], in_=pl[:, :])
                    yield
                    A("dve", V.tensor_reduce, [lg[i]], [s_], out=s_[:, 2:3], in_=lg[i][:, 0:4], axis=AX.X, op=ALU.max)
                    yield
                    A("dve", V.tensor_scalar, [s_], [s_], out=s_[:, 3:4], in0=s_[:, 2:3], scalar1=-1.0, scalar2=None, op0=ALU.mult)
                    yield
                    A("act", ACT.activation, [lg[i], s_], [ge[i], s_], out=ge[i][:, :], in_=lg[i][:, 0:4], func=AF.Exp, bias=s_[:, 3:4], accum_out=s_[:, 4:5])
                    A("dve", V.tensor_scalar, [lg[i], s_], [gm[i]], out=gm[i][:, :], in0=lg[i][:, 0:4], scalar1=s_[:, 2:3], scalar2=None, op0=ALU.is_equal)
                    yield
                    A("dve", V.tensor_scalar, [gm[i]], [gm[i]], out=gm[i][:, :], in0=gm[i][:, :], scalar1=-1.0, scalar2=1e30, op0=ALU.add, op1=ALU.mult)
                    yield
                    A("dve", V.tensor_tensor, [lg[i], gm[i]], [elm[i]], out=elm[i][:, :].rearrange("p (g e) -> p g e", g=4),
                      in0=lg[i][:, 4:36].rearrange("p (g e) -> p g e", g=4), in1=gm[i][:, 0:4].unsqueeze(2).to_broadcast([128, 4, 8]), op=ALU.add)
                    yield
                    A("dve", V.max, [elm[i]], [mx8[i]], out=mx8[i][:, :], in_=elm[i][:, :])
                    yield
                    A("dve", V.tensor_scalar, [elm[i], mx8[i]], [M1[i]], out=M1[i][:, :], in0=elm[i][:, :], scalar1=mx8[i][:, 0:1], scalar2=None, op0=ALU.is_equal)
                    A("dve", V.tensor_scalar, [elm[i], mx8[i]], [M2[i]], out=M2[i][:, :], in0=elm[i][:, :], scalar1=mx8[i][:, 1:2], scalar2=None, op0=ALU.is_equal)
                    A("dve", V.tensor_tensor, [mx8[i]], [s_], out=s_[:, 6:7], in0=mx8[i][:, 0:1], in1=mx8[i][:, 1:2], op=ALU.subtract)
                    yield
                    A("dve", V.tensor_tensor, [M1[i], M2[i]], [Mb[i]], out=Mb[i][:, :], in0=M1[i][:, :], in1=M2[i][:, :], op=ALU.add)
                    A("dve", V.reciprocal, [s_], [s_], out=s_[:, 5:6], in_=s_[:, 4:5])
                    A("act", ACT.activation, [s_], [s_], out=s_[:, 7:8], in_=s_[:, 6:7], func=AF.Sigmoid)
                    yield
                    A("pe", PE.matmul, [ltri, Mb[i]], [prk], out=prk[:, 0, :], lhsT=ltri[:, :], rhs=Mb[i][:, :], start=True, stop=True)
                    A("pe", PE.matmul, [onesb, Mb[i]], [prk], out=prk[:, 1, :], lhsT=onesb[:, :], rhs=Mb[i][:, :], start=False, stop=True, skip_group_check=True)
                    A("dve", V.tensor_tensor, [s_], [wall], out=wall[:, m, 0:1], in0=s_[:, 7:8], in1=s_[:, 5:6], op=ALU.mult)
                    yield
                    A("dve", V.tensor_tensor, [s_, wall], [wall], out=wall[:, m, 1:2], in0=s_[:, 5:6], in1=wall[:, m, 0:1], op=ALU.subtract)
                    A("dve", V.tensor_tensor, [prk, tot], [slot[i]], out=slot[i][:, :], in0=prk[:, 0, :], in1=tot[:, :], op=ALU.add)
                    A("dve", V.tensor_tensor, [prk, tot], [tot], out=tot[:, :], in0=prk[:, 1, :], in1=tot[:, :], op=ALU.add)
                    yield
                    A("dve", V.tensor_scalar, [slot[i]], [ovf[i]], out=ovf[i][:, :], in0=slot[i][:, :], scalar1=float(CAP) - 0.5, scalar2=1e6, op0=ALU.is_ge, op1=ALU.mult)
                    A("dve", V.tensor_tensor, [slot[i], eoff], [slot[i]], out=slot[i][:, :], in0=slot[i][:, :], in1=eoff[:, :], op=ALU.add)
                    yield
                    A("dve", V.tensor_tensor, [slot[i], ovf[i]], [slot[i]], out=slot[i][:, :], in0=slot[i][:, :], in1=ovf[i][:, :], op=ALU.add)
                    yield
                    for k, Mk in enumerate((M1[i], M2[i])):
                        A("dve", V.scalar_tensor_tensor, [Mk, slot[i]], [s_], out=junk[:, 0:NEXP], in0=Mk[:, :], scalar=1.0, in1=slot[i][:, :],
                          op0=ALU.mult, op1=ALU.mult, accum_out=s_[:, 8 + k:9 + k])
                    yield
                    for k in range(2):
                        A("dve", V.tensor_copy, [s_], [dsti], out=dsti[:, m, k:k + 1], in_=s_[:, 8 + k:9 + k])
                    yield
                    for k in range(2):
                        S.add("pool", POOL.indirect_dma_start,
                              dict(out=xsd[:, :], out_offset=bass.IndirectOffsetOnAxis(ap=dsti[:, m, k:k + 1], axis=0), in_=xn[i][:, :], in_offset=None,
                                   bounds_check=bc_reg, oob_is_err=False),
                              [xn[i], dsti], [t_xsd], dma=f"sc{i}")

                gens = [route_tile(m) for m in range(NOWN)]
                active = []
                nxt = 0
                step = 0
                while nxt < NOWN or active:
                    if nxt < NOWN and len(active) < NIL and step % 3 == 0:
                        active.append(gens[nxt])
                        nxt += 1
                    for gI in list(active):
                        try:
                            next(gI)
                        except StopIteration:
                            active.remove(gI)
                    step += 1
                S.barrier()

            with ExitStack() as es:
                xsb2 = [S.tile(es, f"xsb{i}", [128, D], F32) for i in range(2)]
                xsT = S.tile(es, "xsT", [128, 16, CAP], F32R)
                wgu = [S.tile(es, f"wgu{i}", [128, 8, DFF], F32R) for i in range(2)]
                wd = S.tile(es, "wd", [128, 8, D], F32R)
                sg = [S.tile(es, f"sg{i}", [128, CAP], F32) for i in range(8)]
                actT = S.tile(es, "actT", [128, 8, CAP], F32R)
                yb = [S.tile(es, f"yb{i}", [128, D], F32) for i in range(2)]
                ps_t = [S.psum(es, f"p6t{i}", [128, 4, 128], F32) for i in range(2)]
                ps_g = [S.psum(es, f"p6g{i}", [128, 2, CAP], F32) for i in range(4)]
                ps_d = [S.psum(es, f"p6d{i}", [128, 512], F32) for i in range(2)]
                kt_ = kw_ = 0
                for e in range(NEXP):
                    for blk in range(2):
                        xsb = xsb2[blk]
                        DMA(f"xsb{blk}", xsb[:, :], xsd[e * CAP + blk * 128:e * CAP + (blk + 1) * 128, :], [t_xsd], [xsb], q="pool")
                        for q in range(4):
                            bank = ps_t[kt_ % 2]
                            kt_ += 1
                            for k in range(4):
                                c = 4 * q + k
                                A("pe", PE.transpose, [xsb, ident_f], [bank], out=bank[:, k, :], in_=xsb[:, c * 128:(c + 1) * 128], identity=ident_f[:, :])
                            A("dve", V.tensor_tensor, [bank, nf], [xsT], out=xsT[:, 4 * q:4 * q + 4, blk * 128:(blk + 1) * 128], in0=bank[:, :, :],
                              in1=nf[:, 4 * q:4 * q + 4].unsqueeze(2).to_broadcast([128, 4, 128]), op=ALU.mult)
                    for part in range(2):
                        for dh in range(2):
                            w = wgu[kw_ % 2]
                            kw_ += 1
                            DMA("wgu" + w.t.name, w[:, :, :],
                                w_gu[e, dh * 1024:(dh + 1) * 1024, part * DFF:(part + 1) * DFF].rearrange("(c p) n -> p c n", p=128), [], [w])
                            for f in range(8):
                                pg = ps_g[f // 2]
                                for c in range(8):
                                    A("pe", PE.matmul, [w, xsT], [pg], out=pg[:, f % 2, :], lhsT=w[:, c, f * 128:(f + 1) * 128], rhs=xsT[:, dh * 8 + c, :],
                                      start=(dh == 0 and c == 0 and f % 2 == 0), stop=(dh == 1 and c == 7), skip_group_check=True)
                                if dh == 1:
                                    if part == 0:
                                        A("act", ACT.activation, [pg], [sg[f]], out=sg[f][:, :], in_=pg[:, f % 2, :], func=AF.Silu)
                                    else:
                                        A("dve", V.tensor_tensor, [sg[f], pg], [actT], out=actT[:, f, :], in0=sg[f][:, :], in1=pg[:, f % 2, :], op=ALU.mult)
                    DMA("wd", wd[:, :, :], w_dn[e, :, :].rearrange("(c p) n -> p c n", p=128), [], [wd])
                    for cb in range(4):
                        for blk in range(2):
                            acc = ps_d[(cb * 2 + blk) % 2]
                            for f in range(8):
                                A("pe", PE.matmul, [actT, wd], [acc], out=acc[:, :], lhsT=actT[:, f, blk * 128:(blk + 1) * 128], rhs=wd[:, f, cb * 512:(cb + 1) * 512],
                                  start=(f == 0), stop=(f == 7))
                            A("act", ACT.copy, [acc], [yb[blk]], out=yb[blk][:, cb * 512:(cb + 1) * 512], in_=acc[:, :])
                    for blk in range(2):
                        DMA(f"yb{blk}", ysd[e * CAP + blk * 128:e * CAP + (blk + 1) * 128, :], yb[blk][:, :], [yb[blk]], [t_ysd], q="act")
                S.barrier()

            with ExitStack() as es:
                NC4 = 4
                x2 = [S.tile(es, f"x2c{i}", [128, D], F32) for i in range(NC4)]
                y1 = [S.tile(es, f"y1{i}", [128, D], F32) for i in range(NC4)]
                y2 = [S.tile(es, f"y2{i}", [128, D], F32) for i in range(NC4)]

                def load4c(m):
                    if m >= NOWN:
                        return
                    i = m % NC4
                    DMA(f"x2c{i}", x2[i][:, :], out_d[m * 128:(m + 1) * 128, :], [t_out], [x2[i]])
                    for k, yk in enumerate((y1[i], y2[i])):
                        S.add("pool", POOL.indirect_dma_start,
                              dict(out=yk[:, :], out_offset=None, in_=ysd[:, :], in_offset=bass.IndirectOffsetOnAxis(ap=dsti[:, m, k:k + 1], axis=0),
                                   bounds_check=bc_reg, oob_is_err=False),
                              [t_ysd, dsti], [yk], dma=f"yg{k}{i}")

                for m in range(NC4 - 1):
                    load4c(m)
                for m in range(NOWN):
                    i = m % NC4
                    load4c(m + NC4 - 1)
                    A("dve", V.scalar_tensor_tensor, [y1[i], wall, x2[i]], [x2[i]], out=x2[i][:, :], in0=y1[i][:, :], scalar=wall[:, m, 0:1], in1=x2[i][:, :],
                      op0=ALU.mult, op1=ALU.add)
                    A("dve", V.scalar_tensor_tensor, [y2[i], wall, x2[i]], [x2[i]], out=x2[i][:, :], in0=y2[i][:, :], scalar=wall[:, m, 1:2], in1=x2[i][:, :],
                      op0=ALU.mult, op1=ALU.add)
                    DMA(f"x2o{i}", out_d[m * 128:(m + 1) * 128, :], x2[i][:, :], [x2[i]], [t_out], q="act")
                S.barrier()

        nops, nw = S.emit()
        print(f"[build] ops={nops} waits={nw} sems={S.nsem}")
    return nc


def make_in_maps(phases, x, norm_mix, w_in, da_q_norm, da_k_norm, da_lambda_q, da_lambda_k, da_sub_norm,
                 dl_q_norm, dl_k_norm, w_branch_a, w_branch_b, w_out, norm_ffn,
                 w_group_router, w_expert_router, w_gate_up, w_down):
    f = np.float32
    c = make_consts()
    rep = lambda v, n=128: np.ascontiguousarray(np.tile(np.asarray(v, f).reshape(1, -1), (n, 1)))
    pc = lambda v: np.ascontiguousarray(np.asarray(v, f).reshape(16, 128).T)
    shared = dict(c)
    shared.update(
        w_in=np.ascontiguousarray(np.asarray(w_in[0], f)),
        nm=pc(norm_mix[0]), nf=pc(norm_ffn[0]),
        gq_da=rep(da_q_norm[0]), gk_da=rep(da_k_norm[0]), gq_dl=rep(dl_q_norm[0]), gk_dl=rep(dl_k_norm[0]),
        subg=rep(da_sub_norm[0]), lamq=rep(np.asarray(da_lambda_q[0]).reshape(-1)), lamk=rep(np.asarray(da_lambda_k[0]).reshape(-1)),
        w_a=np.ascontiguousarray(np.asarray(w_branch_a[0], f)), w_b=np.ascontiguousarray(np.asarray(w_branch_b[0], f)),
        w_o=np.ascontiguousarray(np.asarray(w_out[0], f)),
        wr=np.ascontiguousarray(np.concatenate([np.asarray(w_group_router[0], f), np.asarray(w_expert_router[0], f)], axis=1)),
    )
    if 4 in phases:
        shared.update(w_gu=np.ascontiguousarray(np.asarray(w_gate_up[0], f)), w_dn=np.ascontiguousarray(np.asarray(w_down[0], f)))
    x = np.asarray(x, f)
    maps = []
    for core in range(8):
        b, j = core // 4, core % 4
        xs = np.zeros((NT * 128, D), f)
        valid = np.zeros((NT, 128), f)
        sh = OWN0 - 16 * j
        xs[sh * 128:] = x[b, :(NT - sh) * 128]
        valid[sh:] = 1.0
        m = dict(shared)
        m["xs"] = xs
        m["valid"] = np.ascontiguousarray(valid.T)
        maps.append(m)
    return maps


def assemble(results):
    out = np.zeros((2, S_LEN, D), np.float32)
    for core in range(8):
        b, j = core // 4, core % 4
        o = np.asarray(results[core]["out"]).reshape(NOWN, 128, D)
        for m in range(NOWN):
            st = 16 * j + m
            out[b, st * 128:(st + 1) * 128] = o[m]
    return out


def kernel(**inputs):
    nc = build()
    maps = make_in_maps((1, 2, 3, 4), **inputs)
    res = run_bass_kernel_spmd(nc, maps, core_ids=list(range(8)))
    return assemble(res.results)
```

```python
import math
from contextlib import ExitStack

import ml_dtypes
import numpy as np

import concourse.bass as bass
import concourse.mybir as mybir
from concourse.bass_utils import run_bass_kernel_spmd

F32 = mybir.dt.float32
F32R = mybir.dt.float32r
BF16 = mybir.dt.bfloat16
I32 = mybir.dt.int32
U32 = mybir.dt.uint32
AF = mybir.ActivationFunctionType
ALU = mybir.AluOpType
AX = mybir.AxisListType

D = 2048
S_LEN = 8192
NT = 64
NOWN = 16
N_IN = 11776
EPS = 1e-6
C_DAQ, C_DAK, C_DAV, C_DLQ, C_DLK, C_DLV, C_GA, C_GB = 0, 1024, 2048, 3072, 4608, 6144, 7680, 9728
NEXP = 32
CAP = 256
DFF = 1024
DL_GROUPS = ((128, 1), (512, 4), (2048, 16))
SAME_ENG_SYNC = True
OWN0 = 48
ALIBI_TH = 44.0
DA_WT = [min(NT, int(math.ceil(ALIBI_TH / (128 * 2.0 ** (-(hh + 1))))) + 1) for hh in range(8)]


class T:
    __slots__ = ("name", "w", "rs")

    def __init__(self, name):
        self.name = name
        self.w = None
        self.rs = []


class Tl:
    def __init__(self, ap, name):
        self.ap = ap
        self.t = T(name)

    def __getitem__(self, k):
        return self.ap[k]


class Op:
    __slots__ = ("eng", "fn", "deps", "sig", "val", "isdma", "sem")


class Sched:
    ENG = ("pe", "act", "dve", "pool", "sp")

    def __init__(self, nc, es):
        self.nc = nc
        self.es = es
        self.e = {"pe": nc.tensor, "act": nc.scalar, "dve": nc.vector, "pool": nc.gpsimd, "sp": nc.sync}
        self.ops = []
        self.last = {k: None for k in self.ENG}
        self.sems = {}
        self.dcount = {}
        self.dma_since = {}
        self.pending = {k: [] for k in self.ENG}
        self.nsem = 0

    def tile(self, es, name, shape, dt):
        ap = es.enter_context(self.nc.sbuf_tensor("sb_" + name, list(shape), dt))
        return Tl(ap, name)

    def psum(self, es, name, shape, dt):
        ap = es.enter_context(self.nc.psum_tensor("ps_" + name, list(shape), dt))
        return Tl(ap, name)

    def dram(self, name):
        return T(name)

    def _sem(self, key):
        if key not in self.sems:
            self.sems[key] = self.es.enter_context(self.nc.semaphore(f"s{self.nsem}"))
            self.nsem += 1
        return self.sems[key]

    def add(self, eng, fn, kw, reads=(), writes=(), dma=None):
        op = Op()
        op.eng = eng
        op.fn = (fn, kw)
        op.sig = False
        op.val = 0
        op.isdma = dma is not None
        op.sem = None
        deps = []
        for r in reads:
            t = r.t if isinstance(r, Tl) else r
            if t.w is not None:
                deps.append(t.w)
        for w in writes:
            t = w.t if isinstance(w, Tl) else w
            if t.w is not None:
                deps.append(t.w)
            deps.extend(t.rs)
        deps.extend(self.pending[eng])
        self.pending[eng] = []
        op.deps = [d for d in dict.fromkeys(deps) if d is not op]
        for r in reads:
            t = r.t if isinstance(r, Tl) else r
            t.rs.append(op)
        for w in writes:
            t = w.t if isinstance(w, Tl) else w
            t.w = op
            t.rs = []
        if op.isdma:
            key = ("dma", dma)
            self._sem(key)
            self.dcount[key] = self.dcount.get(key, 0) + 16
            op.sem = key
            op.val = self.dcount[key]
            self.dma_since[key] = op
        else:
            self.last[eng] = op
        self.ops.append(op)
        return op

    def barrier(self):
        deps = [o for o in self.last.values() if o is not None] + list(self.dma_since.values())
        self.dma_since = {}
        for k in self.ENG:
            self.pending[k] = list(deps)

    def emit(self):
        for op in self.ops:
            for d in op.deps:
                if not d.isdma:
                    if d.eng == op.eng and (d.eng == "pe" or not SAME_ENG_SYNC):
                        continue
                    d.sig = True
        cnt = {k: 0 for k in self.ENG}
        for op in self.ops:
            if not op.isdma and op.sig:
                cnt[op.eng] += 1
                op.val = cnt[op.eng]
                op.sem = ("eng", op.eng)
                self._sem(op.sem)
        waited = {k: {} for k in self.ENG}
        nw = 0
        for op in self.ops:
            E = self.e[op.eng]
            need = {}
            for d in op.deps:
                if not d.isdma and d.eng == op.eng and (d.eng == "pe" or not SAME_ENG_SYNC):
                    continue
                if d.sem is None:
                    continue
                if d.val > need.get(d.sem, 0):
                    need[d.sem] = d.val
            for key, val in need.items():
                if waited[op.eng].get(key, 0) >= val:
                    continue
                waited[op.eng][key] = val
                E.wait_ge(self.sems[key], val)
                nw += 1
            try:
                ins = op.fn[0](**op.fn[1])
            except Exception:
                print("EMIT FAIL", op.eng, getattr(op.fn[0], "__name__", op.fn[0]), {k: (getattr(v, "shape", v), getattr(v, "dtype", None)) for k, v in op.fn[1].items()})
                raise
            if op.isdma:
                ins.then_inc(self.sems[op.sem], 16)
            elif op.sig:
                ins.then_inc(self.sems[op.sem], 1)
        fin = {}
        for op in self.ops:
            if op.sem is not None:
                fin[op.sem] = max(fin.get(op.sem, 0), op.val)
        for key, val in fin.items():
            if waited["sp"].get(key, 0) < val:
                self.nc.sync.wait_ge(self.sems[key], val)
        return len(self.ops), nw


def _bf16(a):
    return np.asarray(a, dtype=np.float32).astype(ml_dtypes.bfloat16)


def make_consts():
    c = {}
    c["ident_f"] = np.eye(128, dtype=np.float32)
    c["ident_b"] = _bf16(np.eye(128))
    kpos = np.arange(S_LEN)
    kb, kr = kpos // 128, kpos % 128
    kaug = np.zeros((8, 4, S_LEN), np.float32)
    for h in range(8):
        sl = 2.0 ** (-(h + 1))
        kaug[h, 0] = sl * 128 * kb
        kaug[h, 1] = sl * kr
        kaug[h, 2] = -sl * 128
        kaug[h, 3] = -sl
    c["kaug"] = _bf16(kaug)
    n = np.arange(NOWN * 128)
    qaug = np.zeros((4, NOWN * 128), np.float32)
    qaug[0] = 1.0
    qaug[1] = 1.0
    qaug[2] = OWN0 + n // 128
    qaug[3] = n % 128
    c["qaug"] = _bf16(qaug)
    dm = np.zeros((128, 4, 512), np.float32)
    ki = np.arange(128)[:, None]
    qi = np.arange(128)[None, :]
    for r in range(4):
        for u in range(4):
            if r > u:
                dm[:, r, u * 128:(u + 1) * 128] = -30000.0
            elif r == u:
                dm[:, r, u * 128:(u + 1) * 128] = np.where(ki <= qi, 0.0, -30000.0)
    c["dmask"] = _bf16(dm)
    tabs = []
    for g, (win, dil) in enumerate(DL_GROUPS):
        no = win // 128 + 1
        for o in range(no):
            dist = 128 * o + qi - ki
            ok = (dist >= 0) & (dist % dil == 0) & (dist <= win)
            tabs.append(np.where(ok, dist, 1e9).astype(np.float32))
    c["distm"] = np.ascontiguousarray(np.stack(tabs, axis=1))
    c["ltri"] = _bf16(np.triu(np.ones((128, 128)), k=1))
    c["ones_b"] = _bf16(np.ones((128, 128)))
    c["eoff"] = np.tile((np.arange(NEXP, dtype=np.float32) * CAP)[None, :], (128, 1))
    return c


DL_SLOPES = [2.0 ** (-8.0 * (h + 1) / 12) for h in range(12)]
DL_NOFF = [w // 128 + 1 for (w, d) in DL_GROUPS]
DL_TBASE = [0, 2, 7]


def build(debug=None, phases=(1, 2, 3, 4)):
    debug = debug or ()
    nc = bass.Bass("TRN2", target_bir_lowering=False)
    nc.dge_precook = False

    def din(name, shape, dt=F32):
        return nc.dram_tensor(name, list(shape), dt, kind="ExternalInput").ap()

    def dscr(name, shape, dt):
        kind = "ExternalOutput" if name in debug else "Internal"
        return nc.dram_tensor(name, list(shape), dt, kind=kind).ap()

    xs = din("xs", [NT * 128, D])
    valid_d = din("valid", [128, NT])
    w_in = din("w_in", [D, N_IN], F32R)
    nm_d = din("nm", [128, 16])
    nf_d = din("nf", [128, 16])
    gq_da_d = din("gq_da", [128, 64])
    gk_da_d = din("gk_da", [128, 64])
    gq_dl_d = din("gq_dl", [128, 128])
    gk_dl_d = din("gk_dl", [128, 128])
    subg_d = din("subg", [128, 128])
    lamq_d = din("lamq", [128, 128])
    lamk_d = din("lamk", [128, 128])
    w_a = din("w_a", [1024, D], F32R)
    w_b = din("w_b", [512, D], F32R)
    w_o = din("w_o", [D, D], F32R)
    wr_d = din("wr", [D, 36])
    if 4 in phases:
        w_gu = din("w_gu", [NEXP, D, 2 * DFF], F32R)
        w_dn = din("w_dn", [NEXP, DFF, D], F32R)
    ident_f_d = din("ident_f", [128, 128])
    ident_b_d = din("ident_b", [128, 128], BF16)
    kaug_d = din("kaug", [8, 4, S_LEN], BF16)
    qaug_d = din("qaug", [4, NOWN * 128], BF16)
    dmask_d = din("dmask", [128, 4, 512], BF16)
    distm_d = din("distm", [128, 24, 128])
    ltri_d = din("ltri", [128, 128], BF16)
    ones_d = din("ones_b", [128, 128], BF16)
    eoff_d = din("eoff", [128, NEXP])

    out_d = nc.dram_tensor("out", [NOWN * 128, D], F32, kind="ExternalOutput").ap()

    kT = dscr("kT", [16, 64, S_LEN], BF16)
    vS = dscr("vS", [8, NT, 128, 130], BF16)
    dkT = dscr("dkT", [12, 128, S_LEN], BF16)
    dvS = dscr("dvS", [12, NT, 128, 130], BF16)
    qT = dscr("qT", [16, 64, NOWN * 128], BF16)
    dqT = dscr("dqT", [12, 128, NOWN * 128], BF16)
    gS = dscr("gS", [NOWN, 128, 4096], F32)
    oA = dscr("oA", [NOWN, 128, 1024], F32)
    oB = dscr("oB", [NOWN, 128, 512], F32)
    mS = dscr("mS", [NOWN, 128, D], F32)
    xsd = dscr("xsd", [NEXP * CAP, D], F32)
    ysd = dscr("ysd", [NEXP * CAP, D], F32)

    es0 = ExitStack()
    with es0:
        S = Sched(nc, es0)
        es0.enter_context(nc.Block())
        E = S.e
        t_kT, t_vS, t_dkT, t_dvS, t_qT, t_dqT = (S.dram(n) for n in ("kT", "vS", "dkT", "dvS", "qT", "dqT"))
        t_gS, t_oA, t_oB, t_mS, t_xsd, t_ysd, t_out = (S.dram(n) for n in ("gS", "oA", "oB", "mS", "xsd", "ysd", "out"))

        def ptile(name, shape, dt=F32):
            return S.tile(es0, name, shape, dt)

        ident_f = ptile("ident_f", [128, 128])
        ident_b = ptile("ident_b", [128, 128], BF16)
        nm = ptile("nm", [128, 16])
        nf = ptile("nf", [128, 16])
        valid = ptile("valid", [128, NT])
        rstd_all = ptile("rstd_all", [128, NT])
        gqk_da = ptile("gqk_da", [128, 64])
        gqk_dl = ptile("gqk_dl", [128, 128])
        subg = ptile("subg", [128, 128])
        neglam = ptile("neglam", [128, 1])
        mhalf = ptile("mhalf", [128, 16])
        junk = ptile("junk", [128, 2048], BF16)
        jt = T("junk_untracked")

        V, ACT, PE, POOL = nc.vector, nc.scalar, nc.tensor, nc.gpsimd

        def A(eng, fn, reads, writes, **kw):
            S.add(eng, fn, kw, reads, writes)

        def DMA(key, out, in_, reads, writes, q="sp"):
            fn = {"sp": nc.sync.dma_start, "act": nc.scalar.dma_start, "pool": nc.gpsimd.dma_start}[q]
            S.add(q, fn, dict(out=out, in_=in_), reads, writes, dma=key)

        def ld(tl, src, key, out=None):
            DMA(key, tl.ap[:] if out is None else out, src, [], [tl])

        ld(ident_f, ident_f_d[:, :], "c0")
        ld(ident_b, ident_b_d[:, :], "c1")
        ld(nm, nm_d[:, :], "c2")
        ld(nf, nf_d[:, :], "c3")
        ld(valid, valid_d[:, :], "c4")
        ld(subg, subg_d[:, :], "c5")
        A("pool", POOL.memset, [], [mhalf], ap=mhalf.ap[:], constant=-0.5)
        with ExitStack() as es:
            ta = S.tile(es, "tmpa", [128, 128], F32)
            tb = S.tile(es, "tmpb", [128, 128], F32)
            e2 = S.tile(es, "tmpe", [128, 2], F32)
            ld(ta, gq_da_d[:, :], "c6", out=ta[:, 0:64])
            ld(tb, gk_da_d[:, :], "c7", out=tb[:, 0:64])
            A("dve", V.scalar_tensor_tensor, [ta, tb], [gqk_da], out=gqk_da.ap[:], in0=ta[:, 0:64], scalar=0.125, in1=tb[:, 0:64],
              op0=ALU.mult, op1=ALU.mult)
            ld(ta, gq_dl_d[:, :], "c6")
            ld(tb, gk_dl_d[:, :], "c7")
            A("dve", V.scalar_tensor_tensor, [ta, tb], [gqk_dl], out=gqk_dl.ap[:], in0=ta[:, :], scalar=128 ** -0.5, in1=tb[:, :],
              op0=ALU.mult, op1=ALU.mult)
            ld(ta, lamq_d[:, :], "c6")
            ld(tb, lamk_d[:, :], "c7")
            for i in range(2):
                A("dve", V.scalar_tensor_tensor, [ta, tb], [e2], out=junk[:, 0:64], in0=ta[:, i * 64:(i + 1) * 64], scalar=1.0,
                  in1=tb[:, i * 64:(i + 1) * 64], op0=ALU.mult, op1=ALU.mult, accum_out=e2[:, i:i + 1])
            A("act", ACT.activation, [e2], [e2], out=e2[:, :], in_=e2[:, :], func=AF.Exp)
            A("dve", V.scalar_tensor_tensor, [e2], [neglam], out=neglam.ap[:], in0=e2[:, 1:2], scalar=-0.2, in1=e2[:, 0:1],
              op0=ALU.add, op1=ALU.subtract)
            A("dve", V.tensor_scalar, [subg], [subg], out=subg.ap[:], in0=subg.ap[:], scalar1=0.8, scalar2=None, op0=ALU.mult)
            S.barrier()

        def rsqrt_ops(ss, rs_ap, n, inv, rs_tl):
            A("dve", V.tensor_scalar, [ss], [ss], out=ss[:, 0:n], in0=ss[:, 0:n], scalar1=inv, scalar2=EPS, op0=ALU.mult, op1=ALU.add)
            A("pool", POOL.tensor_tensor, [ss, mhalf], [rs_tl], out=rs_ap, in0=ss[:, 0:n], in1=mhalf[:, 0:n], op=ALU.pow)

        def sumsq(src_tl, src_ap, n, acc_tl, acc_ap):
            A("dve", V.scalar_tensor_tensor, [src_tl], [acc_tl], out=junk[:, 0:n], in0=src_ap, scalar=1.0, in1=src_ap,
              op0=ALU.mult, op1=ALU.mult, accum_out=acc_ap)

        if 1 in phases:
            with ExitStack() as es:
                TB = 8
                NB3 = 3
                xt = [S.tile(es, f"xt{i}", [128, D], F32) for i in range(2)]
                xT = [S.tile(es, f"xT{i}", [128, 16, 128], F32R) for i in range(TB)]
                wb = [S.tile(es, f"wb{i}", [128, 16, 512], F32R) for i in range(2)]
                pr = [S.tile(es, f"pr{i}", [128, 512], F32) for i in range(NB3)]
                sq = [S.tile(es, f"sq{i}", [128, 512], F32) for i in range(NB3)]
                kn = [S.tile(es, f"kn{i}", [128, 512], BF16) for i in range(NB3)]
                kst = [S.tile(es, f"kst{i}", [128, 8, 128], BF16) for i in range(2)]
                vst = [S.tile(es, f"vst{i}", [128, 4, 130], BF16) for i in range(2)]
                ssq = [S.tile(es, f"ssq{i}", [128, 8], F32) for i in range(NB3)]
                rsq = [S.tile(es, f"rsq{i}", [128, 8], F32) for i in range(NB3)]
                xss = [S.tile(es, f"xss{i}", [128, 1], F32) for i in range(2)]
                ps_mm = [S.psum(es, f"pmm{i}", [128, 512], F32) for i in range(2)]
                ps_tx = [S.psum(es, f"ptx{i}", [128, 4, 128], F32) for i in range(2)]
                ps_tk = [S.psum(es, f"ptk{i}", [128, 8, 128], BF16) for i in range(2)]
                rstd_t = [T(f"rstd{p}") for p in range(NT)]
                for v in vst:
                    A("pool", POOL.memset, [], [v], ap=v.ap[:], constant=0.0)
                ctr = {"x": 0, "w": 0, "mm": 0, "tx": 0, "pp": 0, "tk": 0, "v": 0}

                xseq = [(p, True) for p in range(NT)]
                xbuf = {}

                def issue_x(i):
                    if i >= len(xseq):
                        return
                    xi = xt[i % 2]
                    xbuf[i] = xi
                    p = xseq[i][0]
                    ld(xi, xs[p * 128:(p + 1) * 128, :], "x" + xi.t.name)

                def do_xT(i, slot):
                    p, need_rstd = xseq[i]
                    xi = xbuf[i]
                    if need_rstd:
                        sx = xss[i % 2]
                        sumsq(xi, xi[:, :], D, sx, sx[:, 0:1])
                        rsqrt_ops(sx, rstd_all[:, p:p + 1], 1, 1.0 / D, rstd_t[p])
                    for q in range(4):
                        bank = ps_tx[ctr["tx"] % 2]
                        ctr["tx"] += 1
                        for k in range(4):
                            c = 4 * q + k
                            A("pe", PE.transpose, [xi, ident_f], [bank], out=bank[:, k, :], in_=xi[:, c * 128:(c + 1) * 128], identity=ident_f[:, :])
                        A("dve", V.tensor_tensor, [bank, nm], [xT[slot]], out=xT[slot][:, 4 * q:4 * q + 4, :], in0=bank[:, :, :],
                          in1=nm[:, 4 * q:4 * q + 4].unsqueeze(2).to_broadcast([128, 4, 128]), op=ALU.mult)
                    issue_x(i + 2)

                def mm(slot, w):
                    acc = ps_mm[ctr["mm"] % 2]
                    ctr["mm"] += 1
                    for c in range(16):
                        A("pe", PE.matmul, [xT[slot], w], [acc], out=acc[:, :], lhsT=xT[slot][:, c, :], rhs=w[:, c, :],
                          start=(c == 0), stop=(c == 15))
                    return acc

                def post_norm(acc, p, G, W, gain, dst, dst_t, tok0):
                    i = ctr["pp"] % NB3
                    ctr["pp"] += 1
                    prt, sqt, knt, sst, rst = pr[i], sq[i], kn[i], ssq[i], rsq[i]
                    A("act", ACT.activation, [acc, rstd_t[p]], [prt], out=prt[:, :], in_=acc[:, :], func=AF.Copy, scale=rstd_all[:, p:p + 1])
                    A("pool", POOL.tensor_tensor, [prt], [sqt], out=sqt[:, :], in0=prt[:, :], in1=prt[:, :], op=ALU.mult)
                    A("dve", V.tensor_reduce, [sqt], [sst], out=sst[:, 0:G], in_=sqt[:, :].rearrange("p (g w) -> p g w", g=G), axis=AX.X, op=ALU.add)
                    rsqrt_ops(sst, rst[:, 0:G], G, 1.0 / W, rst)
                    pv = prt[:, :].rearrange("p (g w) -> p g w", g=G)
                    kv = knt[:, :].rearrange("p (g w) -> p g w", g=G)
                    rb = rst[:, 0:G].unsqueeze(2).to_broadcast([128, G, W])
                    if gain is None:
                        A("dve", V.tensor_tensor, [prt, rst], [knt], out=kv, in0=pv, in1=rb, op=ALU.mult)
                    else:
                        A("dve", V.tensor_tensor, [prt, rst], [prt], out=pv, in0=pv, in1=rb, op=ALU.mult)
                        A("pool", POOL.tensor_tensor, [prt, gain], [knt], out=kv, in0=pv,
                          in1=gain[:, 0:W].unsqueeze(1).to_broadcast([128, G, W]), op=ALU.mult)

                    def part_b():
                        j = ctr["tk"] % 2
                        ctr["tk"] += 1
                        kstt, ptk = kst[j], ps_tk[j]
                        for g in range(G):
                            A("pe", PE.transpose, [knt, ident_b], [ptk], out=ptk[0:W, g, :], in_=knt[:, g * W:(g + 1) * W], identity=ident_b[:, :])
                        A("act", ACT.copy, [ptk], [kstt], out=kstt[0:W, 0:G, :], in_=ptk[0:W, 0:G, :])
                        DMA("st" + kstt.t.name, dst[:, :, tok0:tok0 + 128].rearrange("h d t -> d h t"), kstt[0:W, 0:G, :], [kstt], [dst_t], q="act")
                    return part_b

                def post_v(acc, p, dst, dst_t):
                    v = vst[ctr["v"] % 2]
                    ctr["v"] += 1
                    A("act", ACT.activation, [acc, rstd_t[p]], [v], out=v[:, :, 0:128], in_=acc[:, :].rearrange("p (h c) -> p h c", h=4),
                      func=AF.Copy, scale=rstd_all[:, p:p + 1])
                    A("pool", POOL.tensor_copy, [valid], [v], out=v[:, :, 128:129], in_=valid[:, p:p + 1].unsqueeze(1).to_broadcast([128, 4, 1]))
                    DMA("st" + v.t.name, dst[:, p, :, :].rearrange("h p c -> p h c"), v[:, :, :], [v], [dst_t], q="act")
                    return None

                def post_gate(acc, p, m, gb):
                    i = ctr["pp"] % NB3
                    ctr["pp"] += 1
                    prt = pr[i]
                    A("act", ACT.activation, [acc, rstd_t[p]], [prt], out=prt[:, :], in_=acc[:, :], func=AF.Copy, scale=rstd_all[:, p:p + 1])
                    DMA("st" + prt.t.name, gS[m, :, gb * 512:(gb + 1) * 512], prt[:, :], [prt], [t_gS], q="act")
                    return None

                kv_blocks = ([("dak", b_) for b_ in range(2)] + [("dav", b_) for b_ in range(2)] + [("dlk", g) for g in range(3)]
                             + [("dlv", g) for g in range(3)])
                q_blocks = [("daq", b_) for b_ in range(2)] + [("dlq", g) for g in range(3)] + [("gate", g) for g in range(8)]
                col_of = {"dak": C_DAK, "dav": C_DAV, "dlk": C_DLK, "dlv": C_DLV, "daq": C_DAQ, "dlq": C_DLQ, "gate": C_GA}

                def first_tile(kind, bi):
                    if kind in ("dak", "dav"):
                        return max(0, OWN0 - max(DA_WT[4 * bi:4 * bi + 4]))
                    if kind in ("dlk", "dlv"):
                        return OWN0 - (DL_NOFF[bi] - 1)
                    return OWN0
                sbs = []
                for sb in range(NT // TB):
                    tiles = [sb * TB + tl for tl in range(TB)]
                    blocks = [(k_, b_) for (k_, b_) in kv_blocks + q_blocks if first_tile(k_, b_) <= tiles[-1]]
                    sbs.append((tiles, blocks))
                wseq = [(si, kind, bi) for si, (tiles, blocks) in enumerate(sbs) for (kind, bi) in blocks]
                wtile = {}

                def issue_w(i):
                    if i >= len(wseq):
                        return
                    w = wb[i % 2]
                    wtile[i] = w
                    col0 = col_of[wseq[i][1]] + 512 * wseq[i][2]
                    ld(w, w_in[:, col0:col0 + 512].rearrange("(c p) n -> p c n", p=128), "w" + w.t.name)

                pend = []
                issue_x(0)
                issue_x(1)
                issue_w(0)
                for tl in range(TB):
                    do_xT(tl, tl)
                wi = 0
                for si, (tiles, blocks) in enumerate(sbs):
                    for bk, (kind, bi) in enumerate(blocks):
                        w = wtile[wi]
                        issue_w(wi + 1)
                        wi += 1
                        ft = first_tile(kind, bi)
                        for tl, p in enumerate(tiles):
                            m = p - OWN0
                            if p >= ft:
                                acc = mm(tl, w)
                                if len(pend) >= 2:
                                    pb = pend.pop(0)
                                    if pb is not None:
                                        pb()
                                if kind == "dak":
                                    pb = post_norm(acc, p, 8, 64, None, kT[8 * bi:8 * bi + 8], t_kT, p * 128)
                                elif kind == "dlk":
                                    pb = post_norm(acc, p, 4, 128, None, dkT[4 * bi:4 * bi + 4], t_dkT, p * 128)
                                elif kind == "dav":
                                    pb = post_v(acc, p, vS[4 * bi:4 * bi + 4], t_vS)
                                elif kind == "dlv":
                                    pb = post_v(acc, p, dvS[4 * bi:4 * bi + 4], t_dvS)
                                elif kind == "daq":
                                    pb = post_norm(acc, p, 8, 64, gqk_da, qT[8 * bi:8 * bi + 8], t_qT, m * 128)
                                elif kind == "dlq":
                                    pb = post_norm(acc, p, 4, 128, gqk_dl, dqT[4 * bi:4 * bi + 4], t_dqT, m * 128)
                                else:
                                    pb = post_gate(acc, p, m, bi)
                                pend.append(pb)
                            if bk == len(blocks) - 1 and si + 1 < len(sbs):
                                do_xT((si + 1) * TB + tl, tl)
                for pb in pend:
                    if pb is not None:
                        pb()
                S.barrier()

        if 2 in phases:
            with ExitStack() as es:
                kth = [S.tile(es, f"kth{i}", [68, 2, S_LEN], BF16) for i in range(2)]
                qth = [S.tile(es, f"qth{i}", [68, 2, NOWN * 128], BF16) for i in range(2)]
                vh = [S.tile(es, f"vh{i}", [128, NT, 130], BF16) for i in range(2)]
                mk = S.tile(es, "mk", [128, 4, 512], BF16)
                pt = [S.tile(es, f"pt{k}", [128, 2, 512], BF16) for k in range(2)]
                oah = [S.tile(es, f"oah{i}", [128, NOWN, 128], F32) for i in range(2)]
                fr = [S.tile(es, f"fr{i}", [128, 4], F32) for i in range(4)]
                ft = [S.tile(es, f"ft{i}", [128, 128], F32) for i in range(4)]
                fo = [S.tile(es, f"fo{i}", [128, 128], F32) for i in range(4)]
                ps_s = [S.psum(es, f"pss{k}", [128, 2, 512], F32) for k in range(2)]
                ps_o = [S.psum(es, f"pso{k}", [128, 3, 130], F32) for k in range(3)]
                acc_v = [Tl(ps_o[idx // 3][:, idx % 3, :], f"accv{idx}") for idx in range(8)]
                for idx in range(8):
                    acc_v[idx].t = ps_o[idx // 3].t
                ld(mk, dmask_d[:, :, :], "mk")

                def load_head(h):
                    hb = h % 2
                    c0 = max(0, OWN0 - DA_WT[h]) * 128
                    t0 = c0 // 128
                    for i in range(2):
                        DMA(f"kth{hb}", kth[hb][0:64, i, c0:], kT[2 * h + i, :, c0:], [t_kT], [kth[hb]])
                        DMA(f"kth{hb}", kth[hb][64:68, i, c0:], kaug_d[h, :, c0:], [], [kth[hb]])
                        DMA(f"qth{hb}", qth[hb][0:64, i, :], qT[2 * h + i, :, :], [t_qT], [qth[hb]])
                        DMA(f"qth{hb}", qth[hb][64:68, i, :], qaug_d[:, :], [], [qth[hb]])
                    DMA(f"vh{hb}", vh[hb][:, t0:, :], vS[h, t0:].rearrange("t p c -> p t c"), [t_vS], [vh[hb]])

                load_head(0)
                fin = 0
                for h in range(8):
                    hb = h % 2
                    if h + 1 < 8:
                        load_head(h + 1)
                    for a in range(4):
                        d0 = OWN0 + 4 * a
                        lo = max(0, d0 - DA_WT[h])
                        kts = list(range(lo, d0 + 4))

                        def qk(kt):
                            for i in range(2):
                                s_ = ps_s[kt % 2]
                                diag = kt >= d0
                                A("pe", PE.matmul, [kth[hb], qth[hb]], [s_], out=s_[:, i, :], lhsT=kth[hb][0:68, i, kt * 128:(kt + 1) * 128],
                                  rhs=qth[hb][0:68, i, a * 512:(a + 1) * 512], start=True, stop=not diag)
                                if diag:
                                    A("pe", PE.matmul, [ident_b, mk], [s_], out=s_[:, i, :], lhsT=ident_b[:, :], rhs=mk[:, kt - d0, :],
                                      start=False, stop=True)

                        qk(kts[0])
                        for kt in kts:
                            if kt + 1 <= kts[-1]:
                                qk(kt + 1)
                            A("act", ACT.activation, [ps_s[kt % 2]], [pt[kt % 2]], out=pt[kt % 2][:, :, :], in_=ps_s[kt % 2][:, :, :], func=AF.Exp)
                            for i in range(2):
                                for u in range(4):
                                    last = d0 + u
                                    if kt > last:
                                        continue
                                    idx = i * 4 + u
                                    A("pe", PE.matmul, [pt[kt % 2], vh[hb]], [acc_v[idx]], out=acc_v[idx][:, :],
                                      lhsT=pt[kt % 2][:, i, u * 128:(u + 1) * 128], rhs=vh[hb][:, kt, :],
                                      start=(kt == lo and idx % 3 == 0), stop=(kt == last), skip_group_check=True)

                        def fin_chain(u):
                            m = 4 * a + u
                            f = u
                            b0, b1 = acc_v[u], acc_v[4 + u]
                            A("dve", V.reciprocal, [b0], [fr[f]], out=fr[f][:, 0:1], in_=b0[:, 128:129])
                            A("dve", V.reciprocal, [b1], [fr[f]], out=fr[f][:, 1:2], in_=b1[:, 128:129])
                            yield
                            A("dve", V.tensor_scalar, [b1, fr[f], neglam], [ft[f]], out=ft[f][:, :], in0=b1[:, 0:128], scalar1=fr[f][:, 1:2],
                              scalar2=neglam[:, 0:1], op0=ALU.mult, op1=ALU.mult)
                            yield
                            A("dve", V.scalar_tensor_tensor, [b0, fr[f], ft[f]], [fo[f]], out=fo[f][:, :], in0=b0[:, 0:128], scalar=fr[f][:, 0:1],
                              in1=ft[f][:, :], op0=ALU.mult, op1=ALU.add)
                            yield
                            sumsq(fo[f], fo[f][:, :], 128, fr[f], fr[f][:, 2:3])
                            yield
                            A("dve", V.tensor_scalar, [fr[f]], [fr[f]], out=fr[f][:, 2:3], in0=fr[f][:, 2:3], scalar1=1.0 / 128, scalar2=EPS,
                              op0=ALU.mult, op1=ALU.add)
                            yield
                            A("pool", POOL.tensor_tensor, [fr[f], mhalf], [fr[f]], out=fr[f][:, 3:4], in0=fr[f][:, 2:3], in1=mhalf[:, 0:1], op=ALU.pow)
                            yield
                            A("dve", V.scalar_tensor_tensor, [fo[f], fr[f], subg], [oah[hb]], out=oah[hb][:, m, :], in0=fo[f][:, :],
                              scalar=fr[f][:, 3:4], in1=subg[:, :], op0=ALU.mult, op1=ALU.mult)

                        chains = [fin_chain(u) for u in range(4)]
                        while chains:
                            for ch in list(chains):
                                try:
                                    next(ch)
                                except StopIteration:
                                    chains.remove(ch)
                    DMA(f"oah{hb}", oA[:, :, h * 128:(h + 1) * 128].rearrange("m p c -> p m c"), oah[hb][:, :, :], [oah[hb]], [t_oA])
                S.barrier()

            with ExitStack() as es:
                NR = 4
                kd = [S.tile(es, f"kd{g}", [128, S_LEN], BF16) for g in range(3)]
                vd = [S.tile(es, f"vd{g}", [128, NT, 130], BF16) for g in range(3)]
                qd = [S.tile(es, f"qd{g}", [128, NOWN * 128], BF16) for g in range(3)]
                dist = S.tile(es, "dist", [128, 24, 128], F32)
                sc = [S.tile(es, f"sc{i}", [128, 512], F32) for i in range(NR)]
                ptd = [S.tile(es, f"ptd{i}", [128, 512], BF16) for i in range(NR)]
                obh = [S.tile(es, f"obh{i}", [128, NOWN, 128], F32) for i in range(2)]
                frd = [S.tile(es, f"frd{i}", [128, 1], F32) for i in range(4)]
                ps_sd = [S.psum(es, f"psd{i}", [128, 512], F32) for i in range(NR)]
                ps_od = [S.psum(es, f"pod{i}", [128, 130], F32) for i in range(4)]
                ld(dist, distm_d[:, :, :], "dist")
                for h in range(4):
                    hb = h % 2
                    for g in range(3):
                        gh = g * 4 + h
                        DMA(f"kd{g}", kd[g][:, 32 * 128:], dkT[gh, :, 32 * 128:], [t_dkT], [kd[g]])
                        DMA(f"vd{g}", vd[g][:, 32:, :], dvS[gh, 32:].rearrange("t p c -> p t c"), [t_dvS], [vd[g]])
                        DMA(f"qd{g}", qd[g][:, :], dqT[gh, :, :], [t_dqT], [qd[g]])
                    for qg in range(4):
                        p0 = OWN0 + 4 * qg
                        steps = []
                        for g in range(3):
                            no = DL_NOFF[g]
                            for kp in range(p0 - (no - 1), p0 + 4):
                                ulo, uhi = max(0, kp - p0), min(3, kp - p0 + no - 1)
                                steps.append((g, kp, ulo, uhi))
                        first_u, last_u = {}, {}
                        for si_, (g, kp, ulo, uhi) in enumerate(steps):
                            for u in range(ulo, uhi + 1):
                                first_u.setdefault(u, si_)
                                last_u[u] = si_

                        def dqk(si_):
                            g, kp, ulo, uhi = steps[si_]
                            s_ = ps_sd[si_ % NR]
                            A("pe", PE.matmul, [kd[g], qd[g]], [s_], out=s_[:, ulo * 128:(uhi + 1) * 128], lhsT=kd[g][:, kp * 128:(kp + 1) * 128],
                              rhs=qd[g][:, (4 * qg + ulo) * 128:(4 * qg + uhi + 1) * 128], start=True, stop=True)

                        dqk(0)
                        dqk(1)
                        dqk(2)
                        for si_, (g, kp, ulo, uhi) in enumerate(steps):
                            if si_ + 3 < len(steps):
                                dqk(si_ + 3)
                            r_ = si_ % NR
                            nu = uhi - ulo + 1
                            o0 = p0 + ulo - kp
                            A("dve", V.scalar_tensor_tensor, [dist, ps_sd[r_]], [sc[r_]], out=sc[r_][:, ulo * 128:(uhi + 1) * 128].rearrange("p (u c) -> p u c", u=nu),
                              in0=dist[:, DL_TBASE[g] + o0:DL_TBASE[g] + o0 + nu, :], scalar=-DL_SLOPES[g * 4 + h],
                              in1=ps_sd[r_][:, ulo * 128:(uhi + 1) * 128].rearrange("p (u c) -> p u c", u=nu), op0=ALU.mult, op1=ALU.add)
                            A("act", ACT.activation, [sc[r_]], [ptd[r_]], out=ptd[r_][:, ulo * 128:(uhi + 1) * 128], in_=sc[r_][:, ulo * 128:(uhi + 1) * 128],
                              func=AF.Exp)
                            for u in range(ulo, uhi + 1):
                                A("pe", PE.matmul, [ptd[r_], vd[g]], [ps_od[u]], out=ps_od[u][:, :], lhsT=ptd[r_][:, u * 128:(u + 1) * 128], rhs=vd[g][:, kp, :],
                                  start=(first_u[u] == si_), stop=(last_u[u] == si_))
                        for u in range(4):
                            m = 4 * qg + u
                            A("dve", V.reciprocal, [ps_od[u]], [frd[u]], out=frd[u][:, 0:1], in_=ps_od[u][:, 128:129])
                            A("dve", V.tensor_scalar, [ps_od[u], frd[u]], [obh[hb]], out=obh[hb][:, m, :], in0=ps_od[u][:, 0:128], scalar1=frd[u][:, 0:1],
                              scalar2=None, op0=ALU.mult)
                    DMA(f"obh{hb}", oB[:, :, h * 128:(h + 1) * 128].rearrange("m p c -> p m c"), obh[hb][:, :, :], [obh[hb]], [t_oB])
                S.barrier()

        if 3 in phases:
            with ExitStack() as es:
                wa = S.tile(es, "wa", [128, 8, D], F32R)
                wbb = S.tile(es, "wbb", [128, 4, D], F32R)
                ot = [S.tile(es, f"ot{i}", [128, 1536], F32) for i in range(2)]
                oT = [S.tile(es, f"oT{i}", [128, 12, 128], F32R) for i in range(2)]
                gt = [S.tile(es, f"gt{i}", [128, 4096], F32) for i in range(2)]
                mx = [S.tile(es, f"mx{i}", [128, D], F32) for i in range(2)]
                t1 = [S.tile(es, f"t1{i}", [128, 512], F32) for i in range(2)]
                t2 = [S.tile(es, f"t2{i}", [128, 512], F32) for i in range(2)]
                ps_t = [S.psum(es, f"p3t{i}", [128, 4, 128], F32) for i in range(2)]
                ps_a = [S.psum(es, f"p3a{i}", [128, 512], F32) for i in range(2)]
                ps_b = [S.psum(es, f"p3b{i}", [128, 512], F32) for i in range(2)]

                def load3(m):
                    i = m % 2
                    DMA(f"ot{i}", ot[i][:, 0:1024], oA[m, :, :], [t_oA], [ot[i]])
                    DMA(f"ot{i}", ot[i][:, 1024:1536], oB[m, :, :], [t_oB], [ot[i]])
                    DMA(f"gt{i}", gt[i][:, :], gS[m, :, :], [t_gS], [gt[i]])

                def prep3(m):
                    i = m % 2
                    for q in range(3):
                        bank = ps_t[q % 2]
                        for k in range(4):
                            c = 4 * q + k
                            A("pe", PE.transpose, [ot[i], ident_f], [bank], out=bank[:, k, :], in_=ot[i][:, c * 128:(c + 1) * 128], identity=ident_f[:, :])
                        A("act", ACT.copy, [bank], [oT[i]], out=oT[i][:, 4 * q:4 * q + 4, :], in_=bank[:, :, :])
                    A("act", ACT.activation, [gt[i]], [gt[i]], out=gt[i][:, :], in_=gt[i][:, :], func=AF.Sigmoid)

                load3(0)
                load3(1)
                DMA("wa", wa[:, :, :], w_a.rearrange("(c p) n -> p c n", p=128), [], [wa])
                DMA("wbb", wbb[:, :, :], w_b.rearrange("(c p) n -> p c n", p=128), [], [wbb])
                prep3(0)
                k3 = 0
                for m in range(NOWN):
                    i = m % 2
                    if m + 1 < NOWN:
                        prep3(m + 1)
                    for cb in range(4):
                        pa, pb = ps_a[k3 % 2], ps_b[k3 % 2]
                        ta_, tb_ = t1[k3 % 2], t2[k3 % 2]
                        k3 += 1
                        for c in range(8):
                            A("pe", PE.matmul, [oT[i], wa], [pa], out=pa[:, :], lhsT=oT[i][:, c, :], rhs=wa[:, c, cb * 512:(cb + 1) * 512],
                              start=(c == 0), stop=(c == 7))
                        for c in range(4):
                            A("pe", PE.matmul, [oT[i], wbb], [pb], out=pb[:, :], lhsT=oT[i][:, 8 + c, :], rhs=wbb[:, c, cb * 512:(cb + 1) * 512],
                              start=(c == 0), stop=(c == 3))
                        A("dve", V.tensor_tensor, [pa, gt[i]], [ta_], out=ta_[:, :], in0=pa[:, :], in1=gt[i][:, cb * 512:(cb + 1) * 512], op=ALU.mult)
                        A("dve", V.tensor_tensor, [pb, gt[i]], [tb_], out=tb_[:, :], in0=pb[:, :], in1=gt[i][:, 2048 + cb * 512:2048 + (cb + 1) * 512],
                          op=ALU.mult)
                        A("pool", POOL.tensor_tensor, [ta_, tb_], [mx[i]], out=mx[i][:, cb * 512:(cb + 1) * 512], in0=ta_[:, :], in1=tb_[:, :], op=ALU.add)
                    DMA(f"mx{i}", mS[m, :, :], mx[i][:, :], [mx[i]], [t_mS], q="act")
                    if m + 2 < NOWN:
                        load3(m + 2)
                S.barrier()

            with ExitStack() as es:
                TB = 8
                mt = [S.tile(es, f"mt{i}", [128, D], F32) for i in range(2)]
                mT = [S.tile(es, f"mT{i}", [128, 16, 128], F32R) for i in range(TB)]
                wo = [S.tile(es, f"wo{i}", [128, 16, 512], F32R) for i in range(2)]
                xo = [S.tile(es, f"xo{i}", [128, 512], F32) for i in range(3)]
                rs_ = [S.tile(es, f"rs{i}", [128, 512], F32) for i in range(2)]
                ps_t = [S.psum(es, f"p4t{i}", [128, 4, 128], F32) for i in range(2)]
                ps_a = [S.psum(es, f"p4a{i}", [128, 512], F32) for i in range(2)]
                k3 = 0
                wlist = [(sb, cb) for sb in range(NOWN // TB) for cb in range(4)]
                wot = {}

                def issue_wo(i):
                    if i >= len(wlist):
                        return
                    w = wo[i % 2]
                    wot[i] = w
                    cb = wlist[i][1]
                    DMA("wo" + w.t.name, w[:, :, :], w_o[:, cb * 512:(cb + 1) * 512].rearrange("(c p) n -> p c n", p=128), [], [w])

                def load_mt(m):
                    if m < NOWN:
                        DMA(f"mt{m % 2}", mt[m % 2][:, :], mS[m, :, :], [t_mS], [mt[m % 2]])

                issue_wo(0)
                load_mt(0)
                load_mt(1)
                wi = 0
                for sb in range(NOWN // TB):
                    for tl in range(TB):
                        m = sb * TB + tl
                        i = m % 2
                        for q in range(4):
                            bank = ps_t[q % 2]
                            for k in range(4):
                                c = 4 * q + k
                                A("pe", PE.transpose, [mt[i], ident_f], [bank], out=bank[:, k, :], in_=mt[i][:, c * 128:(c + 1) * 128], identity=ident_f[:, :])
                            A("act", ACT.copy, [bank], [mT[tl]], out=mT[tl][:, 4 * q:4 * q + 4, :], in_=bank[:, :, :])
                        load_mt(m + 2)
                    for cb in range(4):
                        w = wot[wi]
                        issue_wo(wi + 1)
                        wi += 1
                        for tl in range(TB):
                            m = sb * TB + tl
                            p = OWN0 + m
                            acc, xo_, r_ = ps_a[k3 % 2], xo[k3 % 3], rs_[k3 % 2]
                            k3 += 1
                            DMA("xo" + xo_.t.name, xo_[:, :], xs[p * 128:(p + 1) * 128, cb * 512:(cb + 1) * 512], [], [xo_])
                            for c in range(16):
                                A("pe", PE.matmul, [mT[tl], w], [acc], out=acc[:, :], lhsT=mT[tl][:, c, :], rhs=w[:, c, :], start=(c == 0), stop=(c == 15))
                            A("dve", V.tensor_tensor, [acc, xo_], [r_], out=r_[:, :], in0=acc[:, :], in1=xo_[:, :], op=ALU.add)
                            DMA("rs" + r_.t.name, out_d[m * 128:(m + 1) * 128, cb * 512:(cb + 1) * 512], r_[:, :], [r_], [t_out], q="act")
                S.barrier()

        if 4 in phases:
            dsti = ptile("dsti", [128, NOWN, 2], I32)
            wall = ptile("wall", [128, NOWN, 2], F32)
            bc_reg = nc.gpsimd.alloc_register("bc")
            S.add("pool", POOL.reg_mov, dict(out_reg=bc_reg, val=NEXP * CAP - 1), [], [])
            with ExitStack() as es:
                NIL = 4
                x2 = [S.tile(es, f"x2{i}", [128, D], F32) for i in range(NIL)]
                xn = [S.tile(es, f"xn{i}", [128, D], F32) for i in range(NIL)]
                xnT = [S.tile(es, f"xnT{i}", [128, 16, 128], F32) for i in range(NIL)]
                wr = S.tile(es, "wr", [128, 16, 36], F32)
                ltri = S.tile(es, "ltri", [128, 128], BF16)
                onesb = S.tile(es, "onesb", [128, 128], BF16)
                eoff = S.tile(es, "eoff", [128, NEXP], F32)
                tot = S.tile(es, "tot", [128, NEXP], F32)
                sm = [S.tile(es, f"sm{i}", [128, 16], F32) for i in range(NIL)]
                lg = [S.tile(es, f"lg{i}", [128, 36], F32) for i in range(NIL)]
                ge = [S.tile(es, f"ge{i}", [128, 4], F32) for i in range(NIL)]
                gm = [S.tile(es, f"gm{i}", [128, 4], F32) for i in range(NIL)]
                elm = [S.tile(es, f"elm{i}", [128, NEXP], F32) for i in range(NIL)]
                mx8 = [S.tile(es, f"mx8{i}", [128, 8], F32) for i in range(NIL)]
                M1 = [S.tile(es, f"M1{i}", [128, NEXP], F32) for i in range(NIL)]
                M2 = [S.tile(es, f"M2{i}", [128, NEXP], F32) for i in range(NIL)]
                Mb = [S.tile(es, f"Mb{i}", [128, NEXP], BF16) for i in range(NIL)]
                slot = [S.tile(es, f"slot{i}", [128, NEXP], F32) for i in range(NIL)]
                ovf = [S.tile(es, f"ovf{i}", [128, NEXP], F32) for i in range(NIL)]
                ps_t = [S.psum(es, f"p5t{i}", [128, 4, 128], F32) for i in range(4)]
                ps_l = [S.psum(es, f"p5l{i}", [128, 36], F32) for i in range(2)]
                ps_r = [S.psum(es, f"p5r{i}", [128, 2, NEXP], F32) for i in range(2)]
                ld(wr, wr_d.rearrange("(c p) n -> p c n", p=128), "wr")
                ld(ltri, ltri_d[:, :], "ltri")
                ld(onesb, ones_d[:, :], "onesb")
                ld(eoff, eoff_d[:, :], "eoff")
                A("dve", V.memset, [], [tot], ap=tot.ap[:], constant=0.0)

                def route_tile(m):
                    i = m % NIL
                    s_ = sm[i]
                    pl, prk = ps_l[m % 2], ps_r[m % 2]
                    DMA(f"x2{i}", x2[i][:, :], out_d[m * 128:(m + 1) * 128, :], [t_out], [x2[i]])
                    sumsq(x2[i], x2[i][:, :], D, s_, s_[:, 0:1])
                    yield
                    A("dve", V.tensor_scalar, [s_], [s_], out=s_[:, 0:1], in0=s_[:, 0:1], scalar1=1.0 / D, scalar2=EPS, op0=ALU.mult, op1=ALU.add)
                    A("pool", POOL.tensor_tensor, [s_, mhalf], [s_], out=s_[:, 1:2], in0=s_[:, 0:1], in1=mhalf[:, 0:1], op=ALU.pow)
                    A("act", ACT.activation, [x2[i], s_], [xn[i]], out=xn[i][:, :], in_=x2[i][:, :], func=AF.Copy, scale=s_[:, 1:2])
                    yield
                    for q in range(4):
                        bank = ps_t[q]
                        for k in range(4):
                            c = 4 * q + k
                            A("pe", PE.transpose, [xn[i], ident_f], [bank], out=bank[:, k, :], in_=xn[i][:, c * 128:(c + 1) * 128], identity=ident_f[:, :])
                        A("dve", V.tensor_tensor, [bank, nf], [xnT[i]], out=xnT[i][:, 4 * q:4 * q + 4, :], in0=bank[:, :, :],
                          in1=nf[:, 4 * q:4 * q + 4].unsqueeze(2).to_broadcast([128, 4, 128]), op=ALU.mult)
                        yield
                    for c in range(16):
                        A("pe", PE.matmul, [xnT[i], wr], [pl], out=pl[:, :], lhsT=xnT[i][:, c, :], rhs=wr[:, c, :], start=(c == 0), stop=(c == 15))
                    A("dve", V.tensor_copy, [pl], [lg[i]], out=lg[i][:, :], in_=pl[:, :])
                    yield
                    A("dve", V.tensor_reduce, [lg[i]], [s_], out=s_[:, 2:3], in_=lg[i][:, 0:4], axis=AX.X, op=ALU.max)
                    yield
                    A("dve", V.tensor_scalar, [s_], [s_], out=s_[:, 3:4], in0=s_[:, 2:3], scalar1=-1.0, scalar2=None, op0=ALU.mult)
                    yield
                    A("act", ACT.activation, [lg[i], s_], [ge[i], s_], out=ge[i][:, :], in_=lg[i][:, 0:4], func=AF.Exp, bias=s_[:, 3:4], accum_out=s_[:, 4:5])
                    A("dve", V.tensor_scalar, [lg[i], s_], [gm[i]], out=gm[i][:, :], in0=lg[i][:, 0:4], scalar1=s_[:, 2:3], scalar2=None, op0=ALU.is_equal)
                    yield
                    A("dve", V.tensor_scalar, [gm[i]], [gm[i]], out=gm[i][:, :], in0=gm[i][:, :], scalar1=-1.0, scalar2=1e30, op0=ALU.add, op1=ALU.mult)
                    yield
                    A("dve", V.tensor_tensor, [lg[i], gm[i]], [elm[i]], out=elm[i][:, :].rearrange("p (g e) -> p g e", g=4),
                      in0=lg[i][:, 4:36].rearrange("p (g e) -> p g e", g=4), in1=gm[i][:, 0:4].unsqueeze(2).to_broadcast([128, 4, 8]), op=ALU.add)
                    yield
                    A("dve", V.max, [elm[i]], [mx8[i]], out=mx8[i][:, :], in_=elm[i][:, :])
                    yield
                    A("dve", V.tensor_scalar, [elm[i], mx8[i]], [M1[i]], out=M1[i][:, :], in0=elm[i][:, :], scalar1=mx8[i][:, 0:1], scalar2=None, op0=ALU.is_equal)
                    A("dve", V.tensor_scalar, [elm[i], mx8[i]], [M2[i]], out=M2[i][:, :], in0=elm[i][:, :], scalar1=mx8[i][:, 1:2], scalar2=None, op0=ALU.is_equal)
                    A("dve", V.tensor_tensor, [mx8[i]], [s_], out=s_[:, 6:7], in0=mx8[i][:, 0:1], in1=mx8[i][:, 1:2], op=ALU.subtract)
                    yield
                    A("dve", V.tensor_tensor, [M1[i], M2[i]], [Mb[i]], out=Mb[i][:, :], in0=M1[i][:, :], in1=M2[i][:, :], op=ALU.add)
                    A("dve", V.reciprocal, [s_], [s_], out=s_[:, 5:6], in_=s_[:, 4:5])
                    A("act", ACT.activation, [s_], [s_], out=s_[:, 7:8], in_=s_[:, 6:7], func=AF.Sigmoid)
                    yield
                    A("pe", PE.matmul, [ltri, Mb[i]], [prk], out=prk[:, 0, :], lhsT=ltri[:, :], rhs=Mb[i][:, :], start=True, stop=True)
                    A("pe", PE.matmul, [onesb, Mb[i]], [prk], out=prk[:, 1, :], lhsT=onesb[:, :], rhs=Mb[i][:, :], start=False, stop=True, skip_group_check=True)
                    A("dve", V.tensor_tensor, [s_], [wall], out=wall[:, m, 0:1], in0=s_[:, 7:8], in1=s_[:, 5:6], op=ALU.mult)
                    yield
                    A("dve", V.tensor_tensor, [s_, wall], [wall], out=wall[:, m, 1:2], in0=s_[:, 5:6], in1=wall[:, m, 0:1], op=ALU.subtract)
                    A("dve", V.tensor_tensor, [prk, tot], [slot[i]], out=slot[i][:, :], in0=prk[:, 0, :], in1=tot[:, :], op=ALU.add)
                    A("dve", V.tensor_tensor, [prk, tot], [tot], out=tot[:, :], in0=prk[:, 1, :], in1=tot[:, :], op=ALU.add)
                    yield
                    A("dve", V.tensor_scalar, [slot[i]], [ovf[i]], out=ovf[i][:, :], in0=slot[i][:, :], scalar1=float(CAP) - 0.5, scalar2=1e6, op0=ALU.is_ge, op1=ALU.mult)
                    A("dve", V.tensor_tensor, [slot[i], eoff], [slot[i]], out=slot[i][:, :], in0=slot[i][:, :], in1=eoff[:, :], op=ALU.add)
                    yield
                    A("dve", V.tensor_tensor, [slot[i], ovf[i]], [slot[i]], out=slot[i][:, :], in0=slot[i][:, :], in1=ovf[i][:, :], op=ALU.add)
                    yield
                    for k, Mk in enumerate((M1[i], M2[i])):
                        A("dve", V.scalar_tensor_tensor, [Mk, slot[i]], [s_], out=junk[:, 0:NEXP], in0=Mk[:, :], scalar=1.0, in1=slot[i][:, :],
                          op0=ALU.mult, op1=ALU.mult, accum_out=s_[:, 8 + k:9 + k])
                    yield
                    for k in range(2):
                        A("dve", V.tensor_copy, [s_], [dsti], out=dsti[:, m, k:k + 1], in_=s_[:, 8 + k:9 + k])
                    yield
                    for k in range(2):
                        S.add("pool", POOL.indirect_dma_start,
                              dict(out=xsd[:, :], out_offset=bass.IndirectOffsetOnAxis(ap=dsti[:, m, k:k + 1], axis=0), in_=xn[i][:, :], in_offset=None,
                                   bounds_check=bc_reg, oob_is_err=False),
                              [xn[i], dsti], [t_xsd], dma=f"sc{i}")

                gens = [route_tile(m) for m in range(NOWN)]
                active = []
                nxt = 0
                step = 0
                while nxt < NOWN or active:
                    if nxt < NOWN and len(active) < NIL and step % 3 == 0:
                        active.append(gens[nxt])
                        nxt += 1
                    for gI in list(active):
                        try:
                            next(gI)
                        except StopIteration:
                            active.remove(gI)
                    step += 1
                S.barrier()

            with ExitStack() as es:
                xsb2 = [S.tile(es, f"xsb{i}", [128, D], F32) for i in range(2)]
                xsT = S.tile(es, "xsT", [128, 16, CAP], F32R)
                wgu = [S.tile(es, f"wgu{i}", [128, 8, DFF], F32R) for i in range(2)]
                wd = S.tile(es, "wd", [128, 8, D], F32R)
                sg = [S.tile(es, f"sg{i}", [128, CAP], F32) for i in range(8)]
                actT = S.tile(es, "actT", [128, 8, CAP], F32R)
                yb = [S.tile(es, f"yb{i}", [128, D], F32) for i in range(2)]
                ps_t = [S.psum(es, f"p6t{i}", [128, 4, 128], F32) for i in range(2)]
                ps_g = [S.psum(es, f"p6g{i}", [128, 2, CAP], F32) for i in range(4)]
                ps_d = [S.psum(es, f"p6d{i}", [128, 512], F32) for i in range(2)]
                kt_ = kw_ = 0
                for e in range(NEXP):
                    for blk in range(2):
                        xsb = xsb2[blk]
                        DMA(f"xsb{blk}", xsb[:, :], xsd[e * CAP + blk * 128:e * CAP + (blk + 1) * 128, :], [t_xsd], [xsb], q="pool")
                        for q in range(4):
                            bank = ps_t[kt_ % 2]
                            kt_ += 1
                            for k in range(4):
                                c = 4 * q + k
                                A("pe", PE.transpose, [xsb, ident_f], [bank], out=bank[:, k, :], in_=xsb[:, c * 128:(c + 1) * 128], identity=ident_f[:, :])
                            A("dve", V.tensor_tensor, [bank, nf], [xsT], out=xsT[:, 4 * q:4 * q + 4, blk * 128:(blk + 1) * 128], in0=bank[:, :, :],
                              in1=nf[:, 4 * q:4 * q + 4].unsqueeze(2).to_broadcast([128, 4, 128]), op=ALU.mult)
                    for part in range(2):
                        for dh in range(2):
                            w = wgu[kw_ % 2]
                            kw_ += 1
                            DMA("wgu" + w.t.name, w[:, :, :],
                                w_gu[e, dh * 1024:(dh + 1) * 1024, part * DFF:(part + 1) * DFF].rearrange("(c p) n -> p c n", p=128), [], [w])
                            for f in range(8):
                                pg = ps_g[f // 2]
                                for c in range(8):
                                    A("pe", PE.matmul, [w, xsT], [pg], out=pg[:, f % 2, :], lhsT=w[:, c, f * 128:(f + 1) * 128], rhs=xsT[:, dh * 8 + c, :],
                                      start=(dh == 0 and c == 0 and f % 2 == 0), stop=(dh == 1 and c == 7), skip_group_check=True)
                                if dh == 1:
                                    if part == 0:
                                        A("act", ACT.activation, [pg], [sg[f]], out=sg[f][:, :], in_=pg[:, f % 2, :], func=AF.Silu)
                                    else:
                                        A("dve", V.tensor_tensor, [sg[f], pg], [actT], out=actT[:, f, :], in0=sg[f][:, :], in1=pg[:, f % 2, :], op=ALU.mult)
                    DMA("wd", wd[:, :, :], w_dn[e, :, :].rearrange("(c p) n -> p c n", p=128), [], [wd])
                    for cb in range(4):
                        for blk in range(2):
                            acc = ps_d[(cb * 2 + blk) % 2]
                            for f in range(8):
                                A("pe", PE.matmul, [actT, wd], [acc], out=acc[:, :], lhsT=actT[:, f, blk * 128:(blk + 1) * 128], rhs=wd[:, f, cb * 512:(cb + 1) * 512],
                                  start=(f == 0), stop=(f == 7))
                            A("act", ACT.copy, [acc], [yb[blk]], out=yb[blk][:, cb * 512:(cb + 1) * 512], in_=acc[:, :])
                    for blk in range(2):
                        DMA(f"yb{blk}", ysd[e * CAP + blk * 128:e * CAP + (blk + 1) * 128, :], yb[blk][:, :], [yb[blk]], [t_ysd], q="act")
                S.barrier()

            with ExitStack() as es:
                NC4 = 4
                x2 = [S.tile(es, f"x2c{i}", [128, D], F32) for i in range(NC4)]
                y1 = [S.tile(es, f"y1{i}", [128, D], F32) for i in range(NC4)]
                y2 = [S.tile(es, f"y2{i}", [128, D], F32) for i in range(NC4)]

                def load4c(m):
                    if m >= NOWN:
                        return
                    i = m % NC4
                    DMA(f"x2c{i}", x2[i][:, :], out_d[m * 128:(m + 1) * 128, :], [t_out], [x2[i]])
                    for k, yk in enumerate((y1[i], y2[i])):
                        S.add("pool", POOL.indirect_dma_start,
                              dict(out=yk[:, :], out_offset=None, in_=ysd[:, :], in_offset=bass.IndirectOffsetOnAxis(ap=dsti[:, m, k:k + 1], axis=0),
                                   bounds_check=bc_reg, oob_is_err=False),
                              [t_ysd, dsti], [yk], dma=f"yg{k}{i}")

                for m in range(NC4 - 1):
                    load4c(m)
                for m in range(NOWN):
                    i = m % NC4
                    load4c(m + NC4 - 1)
                    A("dve", V.scalar_tensor_tensor, [y1[i], wall, x2[i]], [x2[i]], out=x2[i][:, :], in0=y1[i][:, :], scalar=wall[:, m, 0:1], in1=x2[i][:, :],
                      op0=ALU.mult, op1=ALU.add)
                    A("dve", V.scalar_tensor_tensor, [y2[i], wall, x2[i]], [x2[i]], out=x2[i][:, :], in0=y2[i][:, :], scalar=wall[:, m, 1:2], in1=x2[i][:, :],
                      op0=ALU.mult, op1=ALU.add)
                    DMA(f"x2o{i}", out_d[m * 128:(m + 1) * 128, :], x2[i][:, :], [x2[i]], [t_out], q="act")
                S.barrier()

        nops, nw = S.emit()
        print(f"[build] ops={nops} waits={nw} sems={S.nsem}")
    return nc


def make_in_maps(phases, x, norm_mix, w_in, da_q_norm, da_k_norm, da_lambda_q, da_lambda_k, da_sub_norm,
                 dl_q_norm, dl_k_norm, w_branch_a, w_branch_b, w_out, norm_ffn,
                 w_group_router, w_expert_router, w_gate_up, w_down):
    f = np.float32
    c = make_consts()
    rep = lambda v, n=128: np.ascontiguousarray(np.tile(np.asarray(v, f).reshape(1, -1), (n, 1)))
    pc = lambda v: np.ascontiguousarray(np.asarray(v, f).reshape(16, 128).T)
    shared = dict(c)
    shared.update(
        w_in=np.ascontiguousarray(np.asarray(w_in[0], f)),
        nm=pc(norm_mix[0]), nf=pc(norm_ffn[0]),
        gq_da=rep(da_q_norm[0]), gk_da=rep(da_k_norm[0]), gq_dl=rep(dl_q_norm[0]), gk_dl=rep(dl_k_norm[0]),
        subg=rep(da_sub_norm[0]), lamq=rep(np.asarray(da_lambda_q[0]).reshape(-1)), lamk=rep(np.asarray(da_lambda_k[0]).reshape(-1)),
        w_a=np.ascontiguousarray(np.asarray(w_branch_a[0], f)), w_b=np.ascontiguousarray(np.asarray(w_branch_b[0], f)),
        w_o=np.ascontiguousarray(np.asarray(w_out[0], f)),
        wr=np.ascontiguousarray(np.concatenate([np.asarray(w_group_router[0], f), np.asarray(w_expert_router[0], f)], axis=1)),
    )
    if 4 in phases:
        shared.update(w_gu=np.ascontiguousarray(np.asarray(w_gate_up[0], f)), w_dn=np.ascontiguousarray(np.asarray(w_down[0], f)))
    x = np.asarray(x, f)
    maps = []
    for core in range(8):
        b, j = core // 4, core % 4
        xs = np.zeros((NT * 128, D), f)
        valid = np.zeros((NT, 128), f)
        sh = OWN0 - 16 * j
        xs[sh * 128:] = x[b, :(NT - sh) * 128]
        valid[sh:] = 1.0
        m = dict(shared)
        m["xs"] = xs
        m["valid"] = np.ascontiguousarray(valid.T)
        maps.append(m)
    return maps


def assemble(results):
    out = np.zeros((2, S_LEN, D), np.float32)
    for core in range(8):
        b, j = core // 4, core % 4
        o = np.asarray(results[core]["out"]).reshape(NOWN, 128, D)
        for m in range(NOWN):
            st = 16 * j + m
            out[b, st * 128:(st + 1) * 128] = o[m]
    return out


def kernel(**inputs):
    nc = build()
    maps = make_in_maps((1, 2, 3, 4), **inputs)
    res = run_bass_kernel_spmd(nc, maps, core_ids=list(range(8)))
    return assemble(res.results)
```
